# Optimizing a Trainium2 kernel written in Bass

```python
import jax, jax.numpy as jnp
from jax import lax
import numpy as np

D_MODEL = 1024
BATCH = 32
SEQ = 2048
DEPTH = 1

MEM_LEN = 256
BLOCK = 128
WINDOW = 128
EPS = 1e-6
DA_HEADS = 8
DA_HEAD_DIM = 64
DA_V_DIM = 2 * DA_HEAD_DIM
WA_HEADS = 16
WA_KV_HEADS = 4
WA_HEAD_DIM = 64
XA_HEADS = 4
XA_HEAD_DIM = 256
N_BRANCH = 3
DA_QK_W = DA_HEADS * 2 * DA_HEAD_DIM
DA_V_W = DA_HEADS * DA_V_DIM
WA_Q_W = WA_HEADS * WA_HEAD_DIM
WA_KV_W = WA_KV_HEADS * WA_HEAD_DIM
XA_W = XA_HEADS * XA_HEAD_DIM
GATE_W = N_BRANCH * D_MODEL
IN_WIDTHS = (DA_QK_W, DA_QK_W, DA_V_W, WA_Q_W, WA_KV_W, WA_KV_W, XA_W, GATE_W)
IN_W = sum(IN_WIDTHS)
N_EXPERTS = 16
EC_FACTOR = 2
D_EXPERT = 2048
LAMBDA_STD = 0.1

kernel_name = 'hybrid_diffattn_swa_memxattn_ecmoe'


def rmsnorm(x, g):
    xf = x.astype(jnp.float32)
    y = xf * lax.rsqrt(jnp.mean(xf * xf, axis=-1, keepdims=True) + EPS)
    return (y * g.astype(jnp.float32)).astype(x.dtype)


def alibi_slopes(n_heads):
    return jnp.exp2(-8.0 * jnp.arange(1, n_heads + 1, dtype=jnp.float32) / n_heads)


def lambda_init(layer):
    return 0.8 - 0.6 * float(np.exp(-0.3 * layer))


def diff_attention(q, k, v, lam, subln_g, lam_init):
    b, s = q.shape[0], q.shape[1]
    nb = s // BLOCK
    scale = DA_HEAD_DIM ** -0.5
    slopes = alibi_slopes(DA_HEADS)[None, :, None, None, None]
    kpos = jnp.arange(s)
    qb = jnp.moveaxis(q.reshape(b, nb, BLOCK, DA_HEADS, 2, DA_HEAD_DIM), 1, 0)

    def one_block(args):
        qi, i = args
        sc = jnp.einsum('bqhmd,bkhmd->bhmqk', qi, k).astype(jnp.float32) * scale
        qpos = i * BLOCK + jnp.arange(BLOCK)
        dist = jnp.abs(qpos[:, None] - kpos[None, :]).astype(jnp.float32)
        p = jax.nn.softmax(sc - slopes * dist, axis=-1)
        a = p[:, :, 0] - lam * p[:, :, 1]
        return jnp.einsum('bhqk,bkhe->bqhe', a.astype(v.dtype), v)

    o = lax.map(one_block, (qb, jnp.arange(nb)))
    o = jnp.moveaxis(o, 0, 1).reshape(b, s, DA_HEADS, DA_V_DIM)
    o = rmsnorm(o, subln_g) * (1.0 - lam_init)
    return o.reshape(b, s, DA_V_W)


def window_attention(q, k, v, sink):
    b, s = q.shape[0], q.shape[1]
    nb = s // BLOCK
    span = BLOCK + 2 * WINDOW
    groups = WA_HEADS // WA_KV_HEADS
    scale = WA_HEAD_DIM ** -0.5
    slopes = alibi_slopes(WA_HEADS).reshape(WA_KV_HEADS, groups)[None, :, :, None, None]
    sink_logit = sink.astype(jnp.float32).reshape(WA_KV_HEADS, groups)[None, :, :, None, None]
    pad = ((0, 0), (WINDOW, WINDOW), (0, 0), (0, 0))
    kp = jnp.pad(k, pad)
    vp = jnp.pad(v, pad)
    qb = jnp.moveaxis(q.reshape(b, nb, BLOCK, WA_KV_HEADS, groups, WA_HEAD_DIM), 1, 0)

    def one_block(args):
        qi, i = args
        start = i * BLOCK
        kb = lax.dynamic_slice_in_dim(kp, start, span, axis=1)
        vb = lax.dynamic_slice_in_dim(vp, start, span, axis=1)
        sc = jnp.einsum('bqhgd,bkhd->bhgqk', qi, kb).astype(jnp.float32) * scale
        qpos = start + jnp.arange(BLOCK)
        kpos = start - WINDOW + jnp.arange(span)
        dist = jnp.abs(qpos[:, None] - kpos[None, :])
        valid = (dist <= WINDOW) & ((kpos >= 0) & (kpos < s))[None, :]
        sc = jnp.where(valid, sc - slopes * dist.astype(jnp.float32), -jnp.inf)
        sinks = jnp.broadcast_to(sink_logit, sc.shape[:-1] + (1,))
        p = jax.nn.softmax(jnp.concatenate([sc, sinks], axis=-1), axis=-1)[..., :span]
        return jnp.einsum('bhgqk,bkhd->bqhgd', p.astype(vb.dtype), vb)

    o = lax.map(one_block, (qb, jnp.arange(nb)))
    return jnp.moveaxis(o, 0, 1).reshape(b, s, WA_Q_W)


def memory_attention(q, mk, mv):
    sc = jnp.einsum('bqhd,bkhd->bhqk', q, mk).astype(jnp.float32) * (XA_HEAD_DIM ** -0.5)
    p = jax.nn.softmax(sc, axis=-1)
    o = jnp.einsum('bhqk,bkhd->bqhd', p.astype(mv.dtype), mv)
    return o.reshape(o.shape[0], o.shape[1], XA_W)


def expert_choice_ffn(h, w_router, w_gate, w_up, w_down):
    b, s, _ = h.shape
    cap = max(1, EC_FACTOR * s // N_EXPERTS)
    logits = jnp.einsum('bsd,de->bse', h, w_router).astype(jnp.float32)
    aff = jax.nn.softmax(logits, axis=-1)
    g, idx = lax.top_k(jnp.swapaxes(aff, 1, 2), cap)
    bidx = jnp.arange(b)[:, None, None]
    xe = h[bidx, idx]
    a = jnp.einsum('becd,edf->becf', xe, w_gate)
    u = jnp.einsum('becd,edf->becf', xe, w_up)
    ye = jnp.einsum('becf,efd->becd', jax.nn.silu(a) * u, w_down)
    ye = ye * g[..., None].astype(ye.dtype)
    return jnp.zeros_like(h).at[bidx, idx].add(ye)


def hybrid_layer(x, mem, layer, attn_norm_g, mem_norm_g, w_in, w_mem_kv,
                 da_lambda_q1, da_lambda_k1, da_lambda_q2, da_lambda_k2, da_subln_g,
                 wa_sink, w_da_o, w_wa_o, w_xa_o, w_out, ffn_norm_g,
                 w_router, w_exp_gate, w_exp_up, w_exp_down):
    b, s, d = x.shape
    h = rmsnorm(x, attn_norm_g)
    m = rmsnorm(mem, mem_norm_g)
    proj = h @ w_in
    offsets = np.cumsum(IN_WIDTHS)[:-1].tolist()
    da_q, da_k, da_v, wa_q, wa_k, wa_v, xa_q, gates = jnp.split(proj, offsets, axis=-1)

    lam_init = lambda_init(layer)
    lam = (jnp.exp(jnp.sum(da_lambda_q1.astype(jnp.float32) * da_lambda_k1.astype(jnp.float32)))
           - jnp.exp(jnp.sum(da_lambda_q2.astype(jnp.float32) * da_lambda_k2.astype(jnp.float32)))
           + lam_init)
    o_da = diff_attention(da_q.reshape(b, s, DA_HEADS, 2, DA_HEAD_DIM),
                          da_k.reshape(b, s, DA_HEADS, 2, DA_HEAD_DIM),
                          da_v.reshape(b, s, DA_HEADS, DA_V_DIM),
                          lam, da_subln_g, lam_init)

    o_wa = window_attention(wa_q.reshape(b, s, WA_HEADS, WA_HEAD_DIM),
                            wa_k.reshape(b, s, WA_KV_HEADS, WA_HEAD_DIM),
                            wa_v.reshape(b, s, WA_KV_HEADS, WA_HEAD_DIM),
                            wa_sink)

    mk, mv = jnp.split(m @ w_mem_kv, 2, axis=-1)
    n_mem = mem.shape[1]
    o_xa = memory_attention(xa_q.reshape(b, s, XA_HEADS, XA_HEAD_DIM),
                            mk.reshape(b, n_mem, XA_HEADS, XA_HEAD_DIM),
                            mv.reshape(b, n_mem, XA_HEADS, XA_HEAD_DIM))

    gate = jax.nn.sigmoid(gates.reshape(b, s, N_BRANCH, d))
    merged = (gate[:, :, 0] * (o_da @ w_da_o)
              + gate[:, :, 1] * (o_wa @ w_wa_o)
              + gate[:, :, 2] * (o_xa @ w_xa_o))
    x = x + merged @ w_out

    h2 = rmsnorm(x, ffn_norm_g)
    return x + expert_choice_ffn(h2, w_router, w_exp_gate, w_exp_up, w_exp_down)


def setup_inputs(seed: int = 0) -> dict:
    key = jax.random.key(seed)
    ks = jax.random.split(key, 24)
    L, D = DEPTH, D_MODEL

    def w(k, shape, fan_in):
        return jax.random.normal(k, shape, jnp.float32) * (fan_in ** -0.5)

    def gain(k, shape):
        return 1.0 + 0.05 * jax.random.normal(k, shape, jnp.float32)

    def small(k, shape, scale):
        return scale * jax.random.normal(k, shape, jnp.float32)

    return {
        'x': jax.random.normal(ks[0], (BATCH, SEQ, D), jnp.float32),
        'mem': jax.random.normal(ks[1], (BATCH, MEM_LEN, D), jnp.float32),
        'attn_norm_g': gain(ks[2], (L, D)),
        'mem_norm_g': gain(ks[3], (L, D)),
        'w_in': w(ks[4], (L, D, IN_W), D),
        'w_mem_kv': w(ks[5], (L, D, 2 * XA_W), D),
        'da_lambda_q1': small(ks[6], (L, DA_HEAD_DIM), LAMBDA_STD),
        'da_lambda_k1': small(ks[7], (L, DA_HEAD_DIM), LAMBDA_STD),
        'da_lambda_q2': small(ks[8], (L, DA_HEAD_DIM), LAMBDA_STD),
        'da_lambda_k2': small(ks[9], (L, DA_HEAD_DIM), LAMBDA_STD),
        'da_subln_g': gain(ks[10], (L, DA_V_DIM)),
        'wa_sink': small(ks[11], (L, WA_HEADS), 0.5),
        'w_da_o': w(ks[12], (L, DA_V_W, D), DA_V_W),
        'w_wa_o': w(ks[13], (L, WA_Q_W, D), WA_Q_W),
        'w_xa_o': w(ks[14], (L, XA_W, D), XA_W),
        'w_out': w(ks[15], (L, D, D), D),
        'ffn_norm_g': gain(ks[16], (L, D)),
        'w_router': w(ks[17], (L, D, N_EXPERTS), D),
        'w_exp_gate': w(ks[18], (L, N_EXPERTS, D, D_EXPERT), D),
        'w_exp_up': w(ks[19], (L, N_EXPERTS, D, D_EXPERT), D),
        'w_exp_down': w(ks[20], (L, N_EXPERTS, D_EXPERT, D), D_EXPERT),
        'final_norm_g': gain(ks[21], (D,)),
    }


def reference(x, mem, attn_norm_g, mem_norm_g, w_in, w_mem_kv,
              da_lambda_q1, da_lambda_k1, da_lambda_q2, da_lambda_k2, da_subln_g,
              wa_sink, w_da_o, w_wa_o, w_xa_o, w_out, ffn_norm_g,
              w_router, w_exp_gate, w_exp_up, w_exp_down, final_norm_g):
    for l in range(DEPTH):
        x = hybrid_layer(x, mem, l, attn_norm_g[l], mem_norm_g[l], w_in[l], w_mem_kv[l],
                         da_lambda_q1[l], da_lambda_k1[l], da_lambda_q2[l], da_lambda_k2[l],
                         da_subln_g[l], wa_sink[l], w_da_o[l], w_wa_o[l], w_xa_o[l], w_out[l],
                         ffn_norm_g[l], w_router[l], w_exp_gate[l], w_exp_up[l], w_exp_down[l])
    return rmsnorm(x, final_norm_g)
```

```python
import numpy as np
import concourse.bass as bass
import concourse.mybir as mybir
from concourse.bass_utils import run_bass_kernel_spmd

F32 = mybir.dt.float32
BF16 = mybir.dt.bfloat16
ALU = mybir.AluOpType
AF = mybir.ActivationFunctionType
AX = mybir.AxisListType

D = 1024
S = 2048
NT = 16
MEM = 256
NE = 16
CAP = 256
DFF = 2048
EPS = 1e-6
C_DAQ, C_DAK, C_DAV, C_WAQ, C_WAK, C_WAV, C_XAQ, C_GATE = 0, 1024, 2048, 3072, 4096, 4352, 4608, 5632
IN_W = 8704
AOFF = 1920
AW = 3968
LAM_INIT = 0.8 - 0.6 * float(np.exp(-0.3 * 0))
ARENA_F32 = 53200


class Buf:
    __slots__ = ("w", "r")

    def __init__(self):
        self.w = None
        self.r = {}


class Builder:
    def __init__(self, nc, sems):
        self.nc = nc
        self.E = {"pe": nc.tensor, "act": nc.scalar, "dve": nc.vector, "pool": nc.gpsimd, "sp": nc.sync}
        self.free_sems = list(sems)
        self.sem = {}
        self.cnt = {}
        self.seen = {e: {} for e in self.E}
        for e in self.E:
            self._newsem(e)

    def _newsem(self, name):
        self.sem[name] = self.free_sems.pop()
        self.cnt[name] = 0

    def _waits(self, eng, reads, writes):
        need = {}
        for b in reads:
            if b.w is not None:
                k, v = b.w
                if need.get(k, 0) < v:
                    need[k] = v
        for b in writes:
            if b.w is not None:
                k, v = b.w
                if need.get(k, 0) < v:
                    need[k] = v
            for k, v in b.r.items():
                if need.get(k, 0) < v:
                    need[k] = v
        seen = self.seen[eng]
        for k, v in need.items():
            if eng == "pe" and k == "pe":
                continue
            if seen.get(k, 0) < v:
                self.E[eng].wait_ge(self.sem[k], v)
                seen[k] = v

    def _mark(self, clock, reads, writes):
        k, v = clock
        for b in reads:
            if b.r.get(k, 0) < v:
                b.r[k] = v
        for b in writes:
            b.w = clock
            b.r = {}

    def op(self, eng, fn, reads=(), writes=(), inc=True):
        self._waits(eng, reads, writes)
        ins = fn()
        if inc:
            self.cnt[eng] += 1
            ins.then_inc(self.sem[eng], 1)
            clock = (eng, self.cnt[eng])
        else:
            clock = (eng, self.cnt[eng] + 1)
        self._mark(clock, reads, writes)
        return ins

    def dma(self, q, out, in_, reads=(), writes=(), sem=None):
        if sem not in self.sem:
            self._newsem(sem)
        self._waits(q, reads, writes)
        ins = self.E[q].dma_start(out=out, in_=in_)
        self.cnt[sem] += 16
        ins.then_inc(self.sem[sem], 16)
        self._mark((sem, self.cnt[sem]), reads, writes)

    def barrier(self, final=False):
        for e in self.E:
            if e == "pool" and not final:
                continue
            seen = self.seen[e]
            for k, v in self.cnt.items():
                if k == e and e == "pe":
                    continue
                if v > 0 and seen.get(k, 0) < v:
                    self.E[e].wait_ge(self.sem[k], v)
                    seen[k] = v

    def mm(self, out, lhsT, rhs, start, stop, reads, writes, inc=None):
        if inc is None:
            inc = stop
        return self.op("pe", lambda: self.nc.tensor.matmul(out, lhsT, rhs, start=start, stop=stop), reads, writes, inc)

    def tr(self, out, in_, ident, reads, writes, inc=True):
        return self.op("pe", lambda: self.nc.tensor.transpose(out, in_, ident), reads, writes, inc)

    def act(self, out, in_, func, reads, writes, **kw):
        return self.op("act", lambda: self.nc.scalar.activation(out=out, in_=in_, func=func, **kw), reads, writes)

    def ts(self, eng, out, in0, s1, s2, op0, op1, reads, writes):
        e = self.E[eng]
        if op1 is None:
            return self.op(eng, lambda: e.tensor_scalar(out, in0, s1, None, op0), reads, writes)
        return self.op(eng, lambda: e.tensor_scalar(out, in0, s1, s2, op0, op1), reads, writes)

    def stt(self, out, in0, scalar, in1, op0, op1, reads, writes, eng="dve"):
        e = self.E[eng]
        return self.op(eng, lambda: e.scalar_tensor_tensor(out, in0, scalar, in1, op0, op1), reads, writes)

    def tt(self, eng, out, in0, in1, op, reads, writes):
        e = self.E[eng]
        return self.op(eng, lambda: e.tensor_tensor(out, in0, in1, op), reads, writes)

    def cp(self, eng, out, in_, reads, writes):
        if eng == "act":
            return self.op("act", lambda: self.nc.scalar.copy(out, in_), reads, writes)
        e = self.E[eng]
        return self.op(eng, lambda: e.tensor_copy(out, in_), reads, writes)


class _Cut(Exception):
    pass


def build_nc(nseq, stop_phase=99, cut=None):
    nc = bass.Bass("TRN2", target_bir_lowering=False)
    dt = lambda name, shape, kind="ExternalInput", d=F32: nc.dram_tensor(name, list(shape), d, kind=kind).ap()
    x = dt("x", [nseq, S, D])
    mem = dt("mem", [nseq, MEM, D])
    w_in = dt("w_in", [D, IN_W])
    w_mem_kv = dt("w_mem_kv", [D, 2 * D])
    w_bo = [dt("w_xa_o", [D, D]), dt("w_da_o", [D, D]), dt("w_wa_o", [D, D])]
    w_out = dt("w_out", [D, D])
    w_router = dt("w_router", [D, NE])
    w_eg = dt("w_exp_gate", [NE, D, DFF])
    w_eu = dt("w_exp_up", [NE, D, DFF])
    w_ed = dt("w_exp_down", [NE, DFF, D])
    gcols_d = dt("gcols", [128, 24])
    vecs_d = dt("vecs", [1, 64 * 4 + 128 + 16])
    gffn_d = dt("gffn", [1, D])
    gfin_d = dt("gfin", [1, D])
    cA_d = dt("cA", [128, AW])
    cI_d = dt("cI", [128, 128])
    cIota_d = dt("cIota", [128, 256])
    cMneg_d = dt("cMneg", [128, 384])
    cCidx_d = dt("cCidx", [128, 2])
    cOneh_d = dt("cOneh", [16, 2048])
    out = dt("out", [nseq, S, D], kind="ExternalOutput")

    from contextlib import ExitStack
    with ExitStack() as es:
        arena = es.enter_context(nc.sbuf_tensor("arena", [128, ARENA_F32], F32))
        psum = es.enter_context(nc.psum_tensor("psum", [128, 4096], F32))
        sems = [es.enter_context(nc.semaphore("s%d" % i)) for i in range(60)]
        B = Builder(nc, sems)

        def reg(off, shape, dtype=F32):
            n = int(np.prod(shape))
            assert off % 4 == 0
            if dtype == F32:
                assert off // 4 + n <= ARENA_F32, (off, shape)
                ap = arena[:, off // 4: off // 4 + n]
            else:
                assert n % 2 == 0 and off // 4 + n // 2 <= ARENA_F32, (off, shape)
                ap = arena[:, off // 4: off // 4 + n // 2].bitcast(BF16)
            if len(shape) == 2:
                return ap.rearrange("p (a b) -> p a b", a=shape[0])
            if len(shape) == 3:
                return ap.rearrange("p (a b c) -> p a b c", a=shape[0], b=shape[1])
            return ap

        def pbank(b, dtype=F32):
            ap = psum[:, b * 512:(b + 1) * 512]
            return ap if dtype == F32 else ap.bitcast(BF16)

        KB = 1024
        cA = reg(0, [AW])
        o = AW * 4
        identF = reg(o, [128]); o += 512
        identB = reg(o, [128], BF16); o += 256
        iota256 = reg(o, [256]); o += 1024
        mneg = reg(o, [3, 128]); o += 1536
        gsub = reg(o, [128]); o += 512
        lamv = reg(o, [4, 64]); o += 1024
        gcols = reg(o, [24]); o += 96
        cidx = reg(o, [2]); o += 8
        esink = reg(o, [16]); o += 64
        lamc = reg(o, [8]); o += 32
        sinkv = reg(o, [16]); o += 64
        wr32 = reg(o, [8, 16]); o += 512
        epsc = reg(o, [4]); o += 16
        wrhi = reg(o, [8, 16], BF16); o += 256
        wrlo = reg(o, [8, 16], BF16); o += 256
        assert o <= 22 * KB, o
        WSLOT = [reg(22 * KB + i * 8 * KB, [8, 512], BF16) for i in range(4)]
        wbuf = [Buf() for _ in range(4)]
        wctr = [0]
        R_M = 54 * KB
        R_H = 118 * KB
        R_O = 150 * KB
        R_T = 182 * KB

        cst = Buf()
        ld = lambda o_, i_: B.dma("sp", o_, i_, (), (cst,), "c")
        ld(cA, cA_d); ld(identF, cI_d); ld(iota256, cIota_d)
        ld(mneg.rearrange("p a b -> p (a b)"), cMneg_d); ld(cidx, cCidx_d); ld(gcols, gcols_d)
        ld(lamv.rearrange("p a b -> p (a b)"), vecs_d[:, 0:256].partition_broadcast(128))
        ld(gsub, vecs_d[:, 256:384].partition_broadcast(128))
        ld(sinkv, vecs_d[:, 384:400].partition_broadcast(128))
        ld(wr32, w_router.rearrange("(c p) n -> p c n", p=128))
        B.cp("dve", identB, identF, (cst,), (cst,))
        B.op("dve", lambda: nc.vector.memset(epsc[:, 0:1], EPS * D), (), (cst,))
        B.op("dve", lambda: nc.vector.memset(epsc[:, 1:2], EPS * 128), (), (cst,))
        B.op("dve", lambda: nc.vector.memset(epsc[:, 2:3], EPS), (), (cst,))
        junk64 = reg(R_T, [64])
        for i in range(2):
            B.tt("dve", junk64, lamv[:, 2 * i, :], lamv[:, 2 * i + 1, :], ALU.mult, (cst,), (cst,))
            B.op("dve", lambda i=i: nc.vector.reduce_sum(lamc[:, i:i + 1], junk64, axis=AX.X), (cst,), (cst,))
        B.act(lamc[:, 0:2], lamc[:, 0:2], AF.Exp, (cst,), (cst,))
        B.tt("dve", lamc[:, 2:3], lamc[:, 0:1], lamc[:, 1:2], ALU.subtract, (cst,), (cst,))
        B.ts("dve", lamc[:, 3:4], lamc[:, 2:3], LAM_INIT, -1.0, ALU.add, ALU.mult, (cst,), (cst,))
        B.act(esink, sinkv, AF.Exp, (cst,), (cst,))
        B.ts("dve", gsub, gsub, 1.0 - LAM_INIT, None, ALU.mult, None, (cst,), (cst,))
        B.cp("dve", wrhi, wr32, (cst,), (cst,))
        B.tt("dve", wr32, wr32, wrhi, ALU.subtract, (cst,), (cst,))
        B.cp("dve", wrlo, wr32, (cst,), (cst,))
        B.barrier()

        def wload(pieces):
            i = wctr[0] % 4
            wctr[0] += 1
            for (src, coff, nchunk) in pieces:
                ncols = src.shape[-1]
                if nchunk == 8:
                    dst = WSLOT[i][:, :, coff:coff + ncols]
                else:
                    dst = WSLOT[i].rearrange("p a b -> p (a b)").rearrange("p (a b) -> p a b", a=16)[:, :, coff:coff + ncols]
                B.dma("pool", dst, src.rearrange("(c p) n -> p c n", p=128), (), (wbuf[i],), "w%d" % i)
            return WSLOT[i], wbuf[i]

        def rms_rstd(ssq, rs, n, reads, writes):
            k = 0 if n == D else 1
            B.act(rs, ssq, AF.Sqrt, tuple(reads) + (cst,), writes, bias=epsc[:, k:k + 1], scale=1.0)
            B.op("dve", lambda: nc.vector.reciprocal(rs, rs), writes, writes)

        def norm_transpose(src_dram_rows, ntile, gcol0, dstT, dstbuf, tmp_off, sp_):
            xt = [reg(tmp_off + i * 4096, [1024]) for i in range(2)]
            xn = [reg(tmp_off + 8192 + i * 2048, [1024], BF16) for i in range(2)]
            sq = reg(tmp_off + 12288, [1024], BF16)
            st = reg(tmp_off + 14336, [8])
            bx = [Buf(), Buf()]; bn = [Buf(), Buf()]; bsq = Buf(); bst = [Buf(), Buf()]
            bp = [Buf(), Buf()]
            for t in range(ntile):
                i = t % 2
                B.dma("sp", xt[i], src_dram_rows[t * 128:(t + 1) * 128, :], (), (bx[i],), sp_ + str(i))
                B.act(sq, xt[i], AF.Square, (bx[i],), (bsq, bst[i]), accum_out=st[:, i:i + 1])
                rms_rstd(st[:, i:i + 1], st[:, 2 + i:3 + i], D, (bst[i],), (bst[i],))
                B.ts("dve", xn[i], xt[i], st[:, 2 + i:3 + i], 32.0, ALU.mult, ALU.mult, (bx[i], bst[i]), (bn[i],))
                pt = pbank(i, BF16).rearrange("p (c n) -> p c n", c=8)
                for c in range(8):
                    B.tr(pt[:, c, :], xn[i][:, c * 128:(c + 1) * 128], identB, (bn[i],), (bp[i],), inc=(c == 7))
                for c in range(8):
                    B.act(dstT[:, c, t * 128:(t + 1) * 128], pt[:, c, :], AF.Copy, (bp[i],), (dstbuf,),
                          scale=gcols[:, gcol0 + c:gcol0 + c + 1])

        def proj_fm(W, wb, wcol, ncol_chunks, srcT, srcbuf, ntok, dst_fn, dstbuf, banks=(6, 7), ctr=[0]):
            pb = [Buf(), Buf()]
            for cc in range(ncol_chunks):
                for t0 in range(0, ntok, 512):
                    n = min(512, ntok - t0)
                    j = ctr[0] % 2; ctr[0] += 1
                    ps = pbank(banks[j])[:, 0:n]
                    for kc in range(8):
                        B.mm(ps, W[:, kc, wcol + cc * 128: wcol + (cc + 1) * 128], srcT[:, kc, t0:t0 + n],
                             kc == 0, kc == 7, (wb, srcbuf), (PB[banks[j]],))
                    B.cp("act", dst_fn(cc, t0, n), ps, (PB[banks[j]],), (dstbuf,))

        PB = [Buf() for _ in range(8)]

        cur_seq = [0]

        def maybe_cut(name):
            if cut == name or cut == name + '@' + str(cur_seq[0]):
                B.barrier()
                print('CUT at', name, B.cnt)
                raise _Cut()

        try:
          for seq in range(nseq):
            cur_seq[0] = seq
            hT = reg(R_H, [8, S], BF16); bhT = Buf()
            oT = reg(R_O, [8, S], BF16); boT = Buf()
            mT_ = reg(R_M, [8, S]); bmerged = Buf()

            maybe_cut('c0')
            norm_transpose(x[seq], NT, 0, hT, bhT, R_M, 'x')
            maybe_cut('p1')
            TM = R_M + 16 * KB
            mT = reg(TM, [8, MEM], BF16); bmT = Buf()
            norm_transpose(mem[seq], 2, 8, mT, bmT, R_M + 44 * KB, 'm')
            mkT = reg(TM + 4 * KB, [8, MEM], BF16); bmk = Buf()
            mv = reg(TM + 8 * KB, [2, 4, 258], BF16); bmv = Buf()
            XQT = reg(TM + 13 * KB, [2, S], BF16); bxq = Buf()
            PTx = [reg(TM + 21 * KB + i * 2048, [2, 512], BF16) for i in range(2)]; bptx = [Buf(), Buf()]
            ox = [reg(TM + 25 * KB + i * 512, [256], BF16) for i in range(2)]; box = [Buf(), Buf()]
            rx = reg(TM + 26 * KB, [4]); brx = Buf()
            B.op("dve", lambda: nc.vector.memset(mv[:, :, :, 256:258], 1.0), (), (bmv,))
            for half in range(2):
                W, wb = wload([(w_mem_kv[:, half * 512:(half + 1) * 512], 0, 8)])
                proj_fm(W, wb, 0, 4, mT, bmT, MEM, lambda cc, t0, n, half=half: mkT[:, half * 4 + cc, t0:t0 + n], bmk)
            for half in range(2):
                W, wb = wload([(w_mem_kv[:, D + half * 512: D + (half + 1) * 512], 0, 8)])
                for mt in range(2):
                    ps = pbank(6 + mt)
                    for kc in range(8):
                        B.mm(ps, mT[:, kc, mt * 128:(mt + 1) * 128], W[:, kc, :], kc == 0, kc == 7, (wb, bmT), (PB[6 + mt],))
                    B.cp("act", mv[:, mt, 2 * half:2 * half + 2, 0:256], ps.rearrange("p (a b) -> p a b", a=2),
                         (PB[6 + mt],), (bmv,))
            pcx = 0
            for hd in range(4):
                W, wb = wload([(w_in[:, C_XAQ + hd * 256: C_XAQ + (hd + 1) * 256], 0, 8)])
                proj_fm(W, wb, 0, 2, hT, bhT, S, lambda cc, t0, n: XQT[:, cc, t0:t0 + n], bxq)
                for tg in range(4):
                    j = tg % 2
                    for mt in range(2):
                        ps = pbank(mt)
                        for dc in range(2):
                            B.mm(ps, mkT[:, hd * 2 + dc, mt * 128:(mt + 1) * 128], XQT[:, dc, tg * 512:(tg + 1) * 512],
                                 dc == 0, dc == 1, (bmk, bxq), (PB[mt],))
                        B.act(PTx[j][:, mt, :], ps, AF.Exp, (PB[mt],), (bptx[j],), scale=1.0 / 16.0)
                    for qs in range(4):
                        k = pcx % 2; pcx += 1
                        acc = psum[:, (2 + k) * 512:(2 + k) * 512 + 257]
                        for mt in range(2):
                            B.mm(acc, PTx[j][:, mt, qs * 128:(qs + 1) * 128], mv[:, mt, hd, 0:257], mt == 0, mt == 1,
                                 (bptx[j], bmv), (PB[2 + k],))
                        B.op("dve", lambda acc=acc, k=k: nc.vector.reciprocal(rx[:, k:k + 1], acc[:, 256:257]),
                             (PB[2 + k],), (brx,))
                        B.act(ox[k], acc[:, 0:256], AF.Copy, (PB[2 + k], brx), (box[k],), scale=rx[:, k:k + 1])
                        pt = pbank(4 + k, BF16)
                        for c in range(2):
                            B.tr(pt[:, c * 128:(c + 1) * 128], ox[k][:, c * 128:(c + 1) * 128], identB, (box[k],),
                                 (PB[4 + k],), inc=(c == 1))
                        qb = tg * 4 + qs
                        B.cp("dve", oT[:, hd * 2:hd * 2 + 2, qb * 128:(qb + 1) * 128],
                             pt[:, 0:256].rearrange("p (c n) -> p c n", c=2), (PB[4 + k],), (boT,))
            B.barrier()

            def branch_out(b):
                sg = [reg(R_T + i * 2048, [512]) for i in range(2)]; bsg = [Buf(), Buf()]
                tmp = [reg(R_T + 4096 + i * 2048, [512]) for i in range(2)]; btmp = [Buf(), Buf()]
                n = 0
                for cc in range(8):
                    W, wb = wload([(w_bo[b][:, cc * 128:(cc + 1) * 128], 0, 8),
                                   (w_in[:, C_GATE + {0: 2, 1: 0, 2: 1}[b] * D + cc * 128: C_GATE + {0: 2, 1: 0, 2: 1}[b] * D + (cc + 1) * 128], 128, 8)])
                    for tg in range(4):
                        j = n % 2; n += 1
                        psP = pbank(j); psG = pbank(2 + j)
                        for kc in range(8):
                            B.mm(psG, W[:, kc, 128:256], hT[:, kc, tg * 512:(tg + 1) * 512], kc == 0, kc == 7, (wb, bhT), (PB[2 + j],))
                        for kc in range(8):
                            B.mm(psP, W[:, kc, 0:128], oT[:, kc, tg * 512:(tg + 1) * 512], kc == 0, kc == 7, (wb, boT), (PB[j],))
                        B.act(sg[j], psG, AF.Sigmoid, (PB[2 + j],), (bsg[j],))
                        dst = mT_[:, cc, tg * 512:(tg + 1) * 512]
                        if b == 0:
                            B.tt("dve", dst, psP, sg[j], ALU.mult, (PB[j], bsg[j]), (bmerged,))
                        else:
                            B.tt("dve", tmp[j], psP, sg[j], ALU.mult, (PB[j], bsg[j]), (btmp[j],))
                            B.tt("dve", dst, dst, tmp[j], ALU.add, (btmp[j], bmerged), (bmerged,))

            maybe_cut('xa')
            branch_out(0)
            B.barrier()
            maybe_cut('bo0')
            if stop_phase >= 2:
                QT = reg(R_T, [S], BF16); bq = Buf()
                KT = reg(R_T + 4 * KB, [S], BF16); bk = Buf()
                VA = reg(R_T + 8 * KB, [NT, 130], BF16); bv = Buf()
                o_ = R_T + 8 * KB + 4160
                PT = [reg(o_ + i * 1024, [512], BF16) for i in range(4)]; bpt = [Buf() for _ in range(4)]
                o_ += 4096
                o1 = [reg(o_ + i * 512, [128]) for i in range(4)]; bo1 = [Buf() for _ in range(4)]
                o2 = [reg(o_ + 2048 + i * 512, [128]) for i in range(4)]; bo2 = [Buf() for _ in range(4)]
                on = [reg(o_ + 4096 + i * 256, [128], BF16) for i in range(4)]; bon = [Buf() for _ in range(4)]
                sqj = [reg(o_ + 5120 + i * 256, [128], BF16) for i in range(2)]; bsqj = [Buf(), Buf()]
                stv4 = [reg(o_ + 5632 + i * 32, [8]) for i in range(4)]; bstv4 = [Buf() for _ in range(4)]
                assert o_ + 5760 <= ARENA_F32 * 4, o_
                B.op("dve", lambda: nc.vector.memset(VA[:, :, 128:130], 1.0), (), (bv,))
                _ab1 = Buf()
                accb = [_ab1 for _ in range(8)]
                accap = [psum[:, 2048 + (i // 3) * 512 + (i % 3) * 160: 2048 + (i // 3) * 512 + (i % 3) * 160 + 129] for i in range(8)]
                pair_n = [0]
                def da_w(h):
                    return wload([(w_in[:, C_DAQ + h * 128: C_DAQ + (h + 1) * 128], 0, 8),
                                  (w_in[:, C_DAK + h * 128: C_DAK + (h + 1) * 128], 128, 8),
                                  (w_in[:, C_DAV + h * 128: C_DAV + (h + 1) * 128], 256, 8)])
                da_next = da_w(0)
                for h in range(8):
                    W, wb = da_next
                    if h + 1 < 8:
                        da_next = da_w(h + 1)
                    proj_fm(W, wb, 0, 1, hT, bhT, S, lambda cc, t0, n: QT[:, t0:t0 + n], bq, banks=(0, 1))
                    proj_fm(W, wb, 128, 1, hT, bhT, S, lambda cc, t0, n: KT[:, t0:t0 + n], bk, banks=(0, 1))
                    for t4 in range(4):
                        ps = pbank(2 + t4 % 2)
                        for tt_ in range(4):
                            t = t4 * 4 + tt_
                            for kc in range(8):
                                B.mm(ps[:, tt_ * 128:(tt_ + 1) * 128], hT[:, kc, t * 128:(t + 1) * 128], W[:, kc, 256:384],
                                     kc == 0, kc == 7, (wb, bhT), (PB[2 + t4 % 2],), inc=(kc == 7 and tt_ == 3))
                        B.cp("act", VA[:, t4 * 4:(t4 + 1) * 4, 0:128], ps.rearrange("p (a b) -> p a b", a=4),
                             (PB[2 + t4 % 2],), (bv,))
                    slope = 2.0 ** (-(h + 1))
                    def qk_mid(qg, kb):
                        st_ = pair_n[0] % 2
                        s0 = 2 * pair_n[0]
                        pair_n[0] += 1
                        for m in range(2):
                            B.mm(pbank(st_ * 2 + m), KT[m * 64:(m + 1) * 64, kb * 128:(kb + 1) * 128],
                                 QT[m * 64:(m + 1) * 64, qg * 512:(qg + 1) * 512], True, True, (bk, bq), (PB[st_ * 2 + m],))
                        off = qg * 512 - kb * 128 + AOFF
                        for m in range(2):
                            s_ = s0 + m
                            B.stt(pbank(st_ * 2 + m), cA[:, off:off + 512], -slope * 8.0, pbank(st_ * 2 + m), ALU.mult, ALU.add,
                                  (PB[st_ * 2 + m],), (PB[st_ * 2 + m],))
                            B.act(PT[s_ % 4], pbank(st_ * 2 + m), AF.Exp, (PB[st_ * 2 + m],), (bpt[s_ % 4],), scale=0.125)
                        return s0

                    def pv(kb, s0):
                        for m in range(2):
                            s_ = s0 + m
                            for qs in range(4):
                                a = m * 4 + qs
                                B.mm(accap[a], PT[s_ % 4][:, qs * 128:(qs + 1) * 128], VA[:, kb, 0:129], kb == 0 and a in (0, 3, 6),
                                     kb == 15 and a in (2, 5, 7), (bpt[s_ % 4], bv), (accb[a],), inc=(qs == 3))

                    def norm(qg):
                        Q4 = range(4)
                        for qs in Q4:
                            B.op("dve", lambda qs=qs: nc.vector.reciprocal(stv4[qs][:, 0:1], accap[qs][:, 128:129]), (accb[qs],), (bstv4[qs],))
                            B.op("dve", lambda qs=qs: nc.vector.reciprocal(stv4[qs][:, 1:2], accap[4 + qs][:, 128:129]), (accb[4 + qs],), (bstv4[qs],))
                        for qs in Q4:
                            B.tt("dve", stv4[qs][:, 1:2], stv4[qs][:, 1:2], lamc[:, 3:4], ALU.mult, (bstv4[qs],), (bstv4[qs],))
                        for qs in Q4:
                            B.act(o1[qs], accap[qs][:, 0:128], AF.Copy, (accb[qs], bstv4[qs]), (bo1[qs],), scale=stv4[qs][:, 0:1])
                        for qs in Q4:
                            B.stt(o2[qs], accap[4 + qs][:, 0:128], stv4[qs][:, 1:2], o1[qs], ALU.mult, ALU.add,
                                  (accb[4 + qs], bstv4[qs], bo1[qs]), (bo2[qs],))
                        for qs in Q4:
                            B.act(sqj[qs % 2], o2[qs], AF.Square, (bo2[qs],), (bsqj[qs % 2], bstv4[qs]), accum_out=stv4[qs][:, 2:3])
                        for qs in Q4:
                            B.act(stv4[qs][:, 3:4], stv4[qs][:, 2:3], AF.Sqrt, (bstv4[qs], cst), (bstv4[qs],), bias=epsc[:, 2:3], scale=1.0 / 128.0)
                        for qs in Q4:
                            B.op("dve", lambda qs=qs: nc.vector.reciprocal(stv4[qs][:, 4:5], stv4[qs][:, 3:4]), (bstv4[qs],), (bstv4[qs],))
                        for qs in Q4:
                            B.stt(on[qs], o2[qs], stv4[qs][:, 4:5], gsub, ALU.mult, ALU.mult, (bo2[qs], bstv4[qs]), (bon[qs],))
                        pt = pbank(7, BF16)
                        for qs in Q4:
                            B.tr(pt[:, qs * 128:(qs + 1) * 128], on[qs], identB, (bon[qs],), (PB[7],), inc=(qs == 3))
                        B.cp("act", oT[:, h, qg * 512:(qg + 1) * 512], pt[:, 0:512], (PB[7],), (boT,))

                    pend = []
                    for qg in range(4):
                        if qg == 0:
                            pend = [qk_mid(0, 0), qk_mid(0, 1)]
                        for kb in range(16):
                            pv(kb, pend.pop(0))
                            if kb + 2 < 16:
                                pend.append(qk_mid(qg, kb + 2))
                        if qg + 1 < 4:
                            pend = [qk_mid(qg + 1, 0), qk_mid(qg + 1, 1)]
                        norm(qg)
                B.barrier()
                maybe_cut('da')
                branch_out(1)
                B.barrier()
                maybe_cut('bo1')
            if stop_phase >= 3:
                QW = reg(R_T, [2, S], BF16); bqw = Buf()
                KW = reg(R_T + 8 * KB, [S], BF16); bkw = Buf()
                VW = reg(R_T + 12 * KB, [NT, 66], BF16); bvw = Buf()
                o_ = R_T + 12 * KB + 2112
                WB_ = reg(o_, [3, 512]); bwb = Buf(); o_ += 6144
                STw = reg(o_, [512]); bstw = Buf(); o_ += 2048
                PTw = [reg(o_ + i * 1024, [512], BF16) for i in range(2)]; bptw = [Buf(), Buf()]; o_ += 2048
                ow2 = [reg(o_ + i * 512, [256], BF16) for i in range(2)]; bow2 = [Buf(), Buf()]; o_ += 1024
                rw2 = [reg(o_ + i * 32, [8]) for i in range(2)]; brw2 = [Buf(), Buf()]; o_ += 64
                assert o_ <= ARENA_F32 * 4, o_
                B.op("dve", lambda: nc.vector.memset(VW[:, :, 64:66], 1.0), (), (bvw,))
                for kv in range(4):
                    W, wb = wload([(w_in[:, C_WAQ + kv * 256: C_WAQ + (kv + 1) * 256], 0, 8),
                                   (w_in[:, C_WAK + kv * 64: C_WAK + (kv + 1) * 64], 256, 8),
                                   (w_in[:, C_WAK + kv * 64: C_WAK + (kv + 1) * 64], 320, 8),
                                   (w_in[:, C_WAV + kv * 64: C_WAV + (kv + 1) * 64], 384, 8)])
                    proj_fm(W, wb, 0, 2, hT, bhT, S, lambda cc, t0, n: QW[:, cc, t0:t0 + n], bqw)
                    proj_fm(W, wb, 256, 1, hT, bhT, S, lambda cc, t0, n: KW[:, t0:t0 + n], bkw)
                    for t8 in range(2):
                        ps = pbank(6 + t8)
                        for tt_ in range(8):
                            t = t8 * 8 + tt_
                            for kc in range(8):
                                B.mm(ps[:, tt_ * 64:(tt_ + 1) * 64], hT[:, kc, t * 128:(t + 1) * 128], W[:, kc, 384:448],
                                     kc == 0, kc == 7, (wb, bhT), (PB[6 + t8],), inc=(kc == 7 and tt_ == 7))
                        B.cp("act", VW[:, t8 * 8:(t8 + 1) * 8, 0:64], ps.rearrange("p (a b) -> p a b", a=8), (PB[6 + t8],), (bvw,))
                    maybe_cut('wproj')
                    for di, dlt in enumerate((-1, 0, 1)):
                        for g in range(4):
                            slope = 2.0 ** (-8.0 * (kv * 4 + g + 1) / 16.0)
                            B.stt(WB_[:, di, ((g % 2) * 2 + g // 2) * 128:((g % 2) * 2 + g // 2 + 1) * 128], cA[:, AOFF + 128 * dlt: AOFF + 128 * dlt + 128], -slope * 8.0,
                                  mneg[:, di, :], ALU.mult, ALU.add, (cst, bptw[0], bptw[1]), (bwb,))
                    wsteps = []
                    for i in range(16):
                        es_ = [e for e in (-1, 0, 1) if 0 <= i + e < 16]
                        for ei, e in enumerate(es_):
                            wsteps.append((i, ei, e, ei == len(es_) - 1))

                    def w_qk_mid(n):
                        i, ei, e, last = wsteps[n]
                        kb = i + e
                        di = 1 - e
                        j = n % 2
                        pse = pbank(j); pso = pbank(6 + j)
                        for g in range(4):
                            pg = (pse, pso)[g % 2]
                            B.mm(pg[:, (g // 2) * 128:(g // 2 + 1) * 128], KW[(g % 2) * 64:(g % 2) * 64 + 64, kb * 128:(kb + 1) * 128],
                                 QW[(g % 2) * 64:(g % 2) * 64 + 64, g // 2, i * 128:(i + 1) * 128], True, True,
                                 (bkw, bqw), (PB[j], PB[6 + j]), inc=(g == 3))
                        B.tt("dve", STw[:, 0:256], pse[:, 0:256], WB_[:, di, 0:256], ALU.add, (PB[j], bwb), (bstw,))
                        B.tt("dve", STw[:, 256:512], pso[:, 0:256], WB_[:, di, 256:512], ALU.add, (PB[6 + j], bwb), (bstw,))
                        B.act(PTw[j], STw, AF.Exp, (bstw,), (bptw[j],), scale=0.125)

                    def w_pv(n):
                        i, ei, e, last = wsteps[n]
                        kb = i + e
                        j = n % 2
                        acc = psum[:, 1024 + (i % 2) * 512:1024 + (i % 2) * 512 + 320].rearrange("p (g d) -> p g d", g=4)[:, :, 0:65]
                        ab = PB[2 + i % 2]
                        for g in range(4):
                            p_ = (g % 2) * 2 + g // 2
                            B.mm(acc[:, g, :], PTw[j][:, p_ * 128:(p_ + 1) * 128], VW[:, kb, 0:65], ei == 0 and g == 0, last and g == 3,
                                 (bptw[j], bvw), (ab,), inc=(g == 3))

                    def w_norm(i):
                        acc = psum[:, 1024 + (i % 2) * 512:1024 + (i % 2) * 512 + 320].rearrange("p (g d) -> p g d", g=4)[:, :, 0:65]
                        ab = PB[2 + i % 2]
                        r_ = rw2[i % 2]; br = brw2[i % 2]
                        o_w = ow2[i % 2]; bo_ = bow2[i % 2]
                        B.tt("dve", r_[:, 0:4], acc[:, :, 64], esink[:, kv * 4:kv * 4 + 4], ALU.add, (ab, cst), (br,))
                        B.op("dve", lambda: nc.vector.reciprocal(r_[:, 4:8], r_[:, 0:4]), (br,), (br,))
                        for g in range(4):
                            B.act(o_w[:, g * 64:(g + 1) * 64], acc[:, g, 0:64], AF.Copy, (ab, br), (bo_,), scale=r_[:, 4 + g:5 + g])
                        pt = pbank(4 + i % 2, BF16)
                        for c in range(2):
                            B.tr(pt[:, c * 128:(c + 1) * 128], o_w[:, c * 128:(c + 1) * 128], identB, (bo_,), (PB[4 + i % 2],), inc=(c == 1))
                        B.cp("act", oT[:, kv * 2:kv * 2 + 2, i * 128:(i + 1) * 128],
                             pt[:, 0:256].rearrange("p (c n) -> p c n", c=2), (PB[4 + i % 2],), (boT,))

                    w_qk_mid(0)
                    for n in range(len(wsteps)):
                        if n + 1 < len(wsteps):
                            w_qk_mid(n + 1)
                        w_pv(n)
                        if wsteps[n][3]:
                            w_norm(wsteps[n][0])
                B.barrier()
                maybe_cut('wa')
                branch_out(2)
                B.barrier()

            x1 = reg(R_H, [NT, D]); bx1 = [Buf() for _ in range(NT)]
            xt = [reg(R_T + i * 4096, [1024]) for i in range(2)]; bxt = [Buf(), Buf()]
            mb = [reg(R_T + 8192 + i * 2048, [8, 128], BF16) for i in range(2)]; bmb = [Buf(), Buf()]
            Wo = []
            for half in range(2):
                Wo.append(wload([(w_out[:, half * 512:(half + 1) * 512], 0, 8)]))
            for t in range(NT):
                i = t % 2
                B.dma("sp", xt[i], x[seq, t * 128:(t + 1) * 128, :], (), (bxt[i],), "x%d" % i)
                B.cp("act", mb[i], mT_[:, :, t * 128:(t + 1) * 128], (bmerged,), (bmb[i],))
                for half in range(2):
                    ps = pbank(2 * i + half)
                    for kc in range(8):
                        B.mm(ps, mb[i][:, kc, :], Wo[half][0][:, kc, :], kc == 0, kc == 7, (bmb[i], Wo[half][1]), (PB[2 * i + half],))
                    B.tt("dve", x1[:, t, half * 512:(half + 1) * 512], ps, xt[i][:, half * 512:(half + 1) * 512], ALU.add,
                         (PB[2 * i + half], bxt[i]), (bx1[t],))
            B.barrier()

            maybe_cut('x1')
            h2 = reg(R_M, [NT, D], BF16); bh2 = Buf()
            posm = reg(R_M + 32 * KB, [S]); bposm = Buf()
            SM = R_M + 40 * KB
            afft = reg(SM, [NT, NE]); baff = Buf()
            aff3 = reg(SM + 1024, [NT * NE, 3], BF16); baff3 = Buf()
            posT = reg(SM + 2560, [NT, NE]); bposT = Buf()
            gsel = reg(SM + 3584, [2]); bg = Buf()
            sst = reg(SM + 3600, [16]); bsst = Buf()
            mx8 = reg(SM + 3664, [8]); bmx = Buf()
            affr = reg(SM + 3712, [NT * NE]); baffr = Buf()
            EA = R_M + 48 * KB
            affE = reg(EA, [S]); baffE = Buf()
            wrk = reg(EA + 8 * KB, [S]); bwrk = Buf()
            cs = [reg(R_T + i * 8 * KB, [S]) for i in range(2)]; bcs = [Buf(), Buf()]
            gbc = reg(R_T, [D]); bgbc = Buf()
            TT = R_T + 8 * KB
            h2f_ = [reg(R_T + 4 * KB, [D]), reg(R_T + 17 * KB, [D])]; bh2f_ = [Buf(), Buf()]
            lo_ = [reg(TT, [D], BF16), reg(R_T + 21 * KB, [D], BF16)]; blo_ = [Buf(), Buf()]
            h2T = [reg(TT + 2048 + i * 2048, [8, 128], BF16) for i in range(2)]; bh2T = [Buf(), Buf()]
            sqx_ = [reg(TT + 6144, [D], BF16), reg(R_T + 23 * KB, [D], BF16)]; bsqx_ = [Buf(), Buf()]
            lg_ = [reg(TT + 8192 + i * 64, [NE]) for i in range(2)]; blg_ = [Buf(), Buf()]
            bsst_ = [Buf(), Buf()]
            assert R_T + 25 * KB <= ARENA_F32 * 4
            B.dma("sp", gbc, gffn_d.partition_broadcast(128), (), (bgbc,), "c")
            for t in range(NT):
                i2 = t % 2
                sb = i2 * 5
                sc = lambda k: sst[:, sb + k:sb + k + 1]
                h2f = h2f_[i2]; bh2f = bh2f_[i2]; lo = lo_[i2]; blo = blo_[i2]; sqx = sqx_[i2]; bsqx = bsqx_[i2]
                lg = lg_[i2]; blg = blg_[i2]; bs = bsst_[i2]
                B.act(sqx, x1[:, t, :], AF.Square, (bx1[t],), (bsqx, bs), accum_out=sc(0))
                rms_rstd(sc(0), sc(1), D, (bs,), (bs,))
                B.ts("dve", sc(1), sc(1), 32.0, None, ALU.mult, None, (bs,), (bs,))
                B.stt(h2f, x1[:, t, :], sc(1), gbc, ALU.mult, ALU.mult, (bx1[t], bs, bgbc), (bh2f,))
                B.cp("act", h2[:, t, :], h2f, (bh2f,), (bh2,))
                B.tt("dve", lo, h2f, h2[:, t, :], ALU.subtract, (bh2f, bh2), (blo,))
                for k2, src in enumerate((h2[:, t, :], lo)):
                    pt = pbank(k2, BF16).rearrange("p (c n) -> p c n", c=8)
                    for c in range(8):
                        B.tr(pt[:, c, :], src[:, c * 128:(c + 1) * 128], identB, (bh2, blo), (PB[k2],), inc=(c == 7))
                    B.cp("act" if k2 == 0 else "dve", h2T[k2], pt, (PB[k2],), (bh2T[k2],))
                ps = psum[:, 1024:1024 + NE]
                combos = [(0, wrhi), (0, wrlo), (1, wrhi)]
                for ci, (k2, wr) in enumerate(combos):
                    for kc in range(8):
                        B.mm(ps, h2T[k2][:, kc, :], wr[:, kc, :], ci == 0 and kc == 0, ci == 2 and kc == 7,
                             (bh2T[k2], cst), (PB[2],))
                B.op("dve", lambda sc=sc, ps=ps: nc.vector.reduce_max(sc(2), ps, axis=AX.X), (PB[2],), (bs,))
                B.ts("dve", sc(2), sc(2), -1.0, None, ALU.mult, None, (bs,), (bs,))
                B.act(lg, ps, AF.Exp, (PB[2], bs), (blg, bs), bias=sc(2), accum_out=sc(3))
                B.op("dve", lambda sc=sc: nc.vector.reciprocal(sc(4), sc(3)), (bs,), (bs,))
                B.ts("dve", afft[:, t, :], lg, sc(4), None, ALU.mult, None, (blg, bs), (baff,))
                pt = psum[0:16, 1536:1536 + 128]
                B.tr(pt, afft[:, t, :], identF, (baff,), (PB[3],))
                B.cp("act", affE[0:16, t * 128:(t + 1) * 128], pt, (PB[3],), (baffE,))
            affv = afft.rearrange("p a b -> p (a b)")
            B.cp("dve", aff3[:, :, 0], affv, (baff,), (baff3,))
            B.tt("dve", affr, affv, aff3[:, :, 0], ALU.subtract, (baff, baff3), (baffr,))
            B.cp("dve", aff3[:, :, 1], affr, (baffr,), (baff3,))
            B.tt("dve", affr, affr, aff3[:, :, 1], ALU.subtract, (baffr, baff3), (baffr,))
            B.cp("dve", aff3[:, :, 2], affr, (baffr,), (baff3,))
            B.barrier()
            E16 = slice(0, 16)
            B.cp("dve", wrk[E16], affE[E16], (baffE,), (bwrk,))
            for it in range(CAP // 8):
                B.op("dve", lambda: nc.vector.max(out=mx8[E16], in_=wrk[E16]), (bwrk,), (bmx,))
                if it < CAP // 8 - 1:
                    B.op("dve", lambda: nc.vector.match_replace(out=wrk[E16], in_to_replace=mx8[E16], in_values=wrk[E16],
                                                                imm_value=-1.0), (bmx, bwrk), (bwrk,))
            B.op("dve", lambda: nc.vector.tensor_reduce(out=sst[E16, 5:6], in_=mx8[E16], axis=AX.X, op=ALU.min), (bmx,), (bsst,))
            B.ts("dve", wrk[E16], affE[E16], sst[E16, 5:6], None, ALU.is_ge, None, (baffE, bsst), (bwrk,))
            B.cp("dve", cs[0][E16], wrk[E16], (bwrk,), (bcs[0],))
            a = 0
            sh = 1
            while sh < S:
                b_ = 1 - a
                B.cp("act", cs[b_][E16, 0:sh], cs[a][E16, 0:sh], (bcs[a],), (bcs[b_],))
                B.tt("dve", cs[b_][E16, sh:S], cs[a][E16, sh:S], cs[a][E16, 0:S - sh], ALU.add, (bcs[a],), (bcs[b_],))
                a = b_
                sh *= 2
            B.tt("dve", posm[E16], cs[a][E16], wrk[E16], ALU.mult, (bcs[a], bwrk), (bposm,))
            B.ts("dve", posm[E16], posm[E16], -1.0, None, ALU.add, None, (bposm,), (bposm,))
            for t in range(NT):
                pt = psum[:, 1536 + (t % 2) * 512: 1536 + (t % 2) * 512 + 16]
                B.tr(pt, posm[E16, t * 128:(t + 1) * 128], identF[0:16, 0:16], (bposm,), (PB[3 + t % 2],))
                B.cp("act", posT[:, t, :], pt, (PB[3 + t % 2],), (bposT,))
            B.barrier()

            maybe_cut('route')
            Sel = reg(EA, [NT, CAP], BF16); bsel = Buf()
            SelT = reg(EA + 8 * KB, [2, S], BF16); bselT = Buf()
            xeT = reg(R_T, [8, CAP], BF16); bxe = Buf()
            actT = reg(R_T + 4 * KB, [16, CAP], BF16); bact = Buf()
            ye = reg(R_T + 12 * KB, [2, D], BF16); bye = Buf()
            oneh = reg(R_T + 16 * KB, [S]); boneh = Buf()
            sa = [reg(SM + 5120 + i * 1024, [CAP]) for i in range(2)]; bsa = [Buf(), Buf()]
            B.dma("sp", oneh[0:16], cOneh_d, (), (boneh,), "c")
            pn = 0
            def build_sel(e):
                for t in range(NT):
                    B.ts("dve", Sel[:, t, :], iota256, posT[:, t, e:e + 1], None, ALU.is_equal, None, (cst, bposT), (bsel,))

            def build_selT(e):
                for tg in range(4):
                    j = tg % 2
                    ps = pbank(j)
                    B.mm(ps, oneh[0:16, e * 128:(e + 1) * 128], posm[0:16, tg * 512:(tg + 1) * 512], True, True, (boneh, bposm), (PB[j],))
                    for cch in range(2):
                        B.ts("dve", SelT[:, cch, tg * 512:(tg + 1) * 512], ps, cidx[:, cch:cch + 1], None, ALU.is_equal, None,
                             (PB[j], cst), (bselT,))

            def gather_group(dc):
                j = dc % 2
                ps = pbank(j)[:, 0:CAP]
                for t in range(NT):
                    B.mm(ps, h2[:, t, dc * 128:(dc + 1) * 128], Sel[:, t, :], t == 0, t == NT - 1, (bh2, bsel), (PB[j],))
                B.cp("act", xeT[:, dc, :], ps, (PB[j],), (bxe,))

            def g_gather(e):
                for cch in range(2):
                    ps = psum[:, 3584 - cch * 512: 3584 - cch * 512 + 3]
                    for t in range(NT):
                        B.mm(ps, Sel[:, t, cch * 128:(cch + 1) * 128], aff3[:, t * NE + e, :], t == 0, t == NT - 1,
                             (bsel, baff3), (PB[7 - cch],), inc=(t == NT - 1))
                    B.op("dve", lambda ps=ps, cch=cch: nc.vector.reduce_sum(gsel[:, cch:cch + 1], ps, axis=AX.X), (PB[7 - cch],), (bg,))

            gu_state = {}

            def gate_up_group(e, fc, fs):
                if fs == 0:
                    gu_state["g"] = wload([(w_eg[e][:, fc * 512:(fc + 1) * 512], 0, 8)])
                    gu_state["u"] = wload([(w_eu[e][:, fc * 512:(fc + 1) * 512], 0, 8)])
                Wg, wbg = gu_state["g"]; Wu, wbu = gu_state["u"]
                j = (fc * 4 + fs) % 2
                ps = pbank(2 + j)
                for kc in range(8):
                    B.mm(ps[:, 0:CAP], Wg[:, kc, fs * 128:(fs + 1) * 128], xeT[:, kc, :], kc == 0, kc == 7, (wbg, bxe), (PB[2 + j],), inc=False)
                for kc in range(8):
                    B.mm(ps[:, CAP:2 * CAP], Wu[:, kc, fs * 128:(fs + 1) * 128], xeT[:, kc, :], kc == 0, kc == 7, (wbu, bxe), (PB[2 + j],))
                B.act(sa[j], ps[:, 0:CAP], AF.Silu, (PB[2 + j],), (bsa[j],))
                B.tt("dve", actT[:, fc * 4 + fs, :], sa[j], ps[:, CAP:2 * CAP], ALU.mult, (bsa[j], PB[2 + j]), (bact,))

            def down(e):
                for dq in range(4):
                    Wd, wbd = wload([(w_ed[e][:, dq * 256:(dq + 1) * 256], 0, 16)])
                    Wd16 = Wd.rearrange("p a b -> p (a b)").rearrange("p (a b) -> p a b", a=16)
                    for cch in range(2):
                        j = (dq * 2 + cch) % 2
                        ps = pbank(4 + j)[:, 0:256]
                        for fch in range(16):
                            B.mm(ps, actT[:, fch, cch * 128:(cch + 1) * 128], Wd16[:, fch, :], fch == 0, fch == 15, (bact, wbd), (PB[4 + j],))
                        B.act(ye[:, cch, dq * 256:(dq + 1) * 256], ps, AF.Copy, (PB[4 + j], bg), (bye,), scale=gsel[:, cch:cch + 1])

            def scatter_unit(ui):
                t, half = ui // 2, ui % 2
                j = ui % 2
                ps = pbank(6 + j)
                for cch in range(2):
                    B.mm(ps, SelT[:, cch, t * 128:(t + 1) * 128], ye[:, cch, half * 512:(half + 1) * 512], cch == 0, cch == 1,
                         (bselT, bye), (PB[6 + j],))
                B.tt("dve", x1[:, t, half * 512:(half + 1) * 512], x1[:, t, half * 512:(half + 1) * 512], ps, ALU.add,
                     (PB[6 + j], bx1[t]), (bx1[t],))

            build_sel(0)
            for dc in range(8):
                gather_group(dc)
            g_gather(0)
            build_selT(0)
            for fc in range(4):
                for fs in range(4):
                    gate_up_group(0, fc, fs)
            if NE > 1:
                build_sel(1)
            for e in range(NE):
                down(e)
                if e + 1 < NE:
                    for dc in range(8):
                        gather_group(dc)
                    g_gather(e + 1)
                    for k in range(16):
                        scatter_unit(2 * k)
                        gate_up_group(e + 1, k // 4, k % 4)
                        scatter_unit(2 * k + 1)
                    build_selT(e + 1)
                    if e + 2 < NE:
                        build_sel(e + 2)
                else:
                    for ui in range(32):
                        scatter_unit(ui)
            B.barrier()
            maybe_cut('moe')
            gfb = reg(R_T, [D]); bgf = Buf()
            ob = [reg(R_T + 4 * KB + i * 4 * KB, [D]) for i in range(2)]; bob = [Buf(), Buf()]
            sq2 = reg(R_T + 12 * KB, [D], BF16); bsq2 = Buf()
            B.dma("sp", gfb, gfin_d.partition_broadcast(128), (), (bgf,), "c")
            for t in range(NT):
                i = t % 2
                B.act(sq2, x1[:, t, :], AF.Square, (bx1[t],), (bsq2, bsst), accum_out=sst[:, 8:9])
                rms_rstd(sst[:, 8:9], sst[:, 9:10], D, (bsst,), (bsst,))
                B.ts("dve", sst[:, 9:10], sst[:, 9:10], 32.0, None, ALU.mult, None, (bsst,), (bsst,))
                B.stt(ob[i], x1[:, t, :], sst[:, 9:10], gfb, ALU.mult, ALU.mult, (bx1[t], bsst, bgf), (bob[i],))
                B.dma("sp", out[seq, t * 128:(t + 1) * 128, :], ob[i], (bob[i],), (), "o%d" % i)
            B.barrier(final=True)
        except _Cut:
            pass
        build_nc.last_counts = dict(B.cnt)
    return nc


def _consts():
    p = np.arange(128, dtype=np.float32)[:, None]
    m = np.arange(AW, dtype=np.float32)[None, :]
    cA = np.abs(m - p - AOFF).astype(np.float32)
    cI = np.eye(128, dtype=np.float32)
    cIota = np.broadcast_to(np.arange(256, dtype=np.float32)[None, :], (128, 256)).copy()
    j = np.arange(128, dtype=np.float32)[None, :]
    mneg = np.zeros((128, 3, 128), np.float32)
    for di, dlt in enumerate((-1, 0, 1)):
        dist = np.abs(j + 128 * dlt - p)
        mneg[:, di, :] = np.where(dist <= 128, 0.0, -1.0e5)
    cCidx = np.stack([np.arange(128, dtype=np.float32), np.arange(128, dtype=np.float32) + 128], axis=1)
    oneh = np.zeros((16, 2048), np.float32)
    for e in range(16):
        oneh[e, e * 128:(e + 1) * 128] = 1.0
    return dict(cA=cA, cI=cI, cIota=cIota, cMneg=mneg.reshape(128, 384), cCidx=np.ascontiguousarray(cCidx), cOneh=oneh)


_NC_CACHE = {}


def _prep_shared(inp):
    f = lambda a: np.ascontiguousarray(np.asarray(a, dtype=np.float32))
    gcols = np.zeros((128, 24), np.float32)
    gcols[:, 0:8] = f(inp["attn_norm_g"])[0].reshape(8, 128).T
    gcols[:, 8:16] = f(inp["mem_norm_g"])[0].reshape(8, 128).T
    vecs = np.concatenate([f(inp["da_lambda_q1"])[0], f(inp["da_lambda_k1"])[0], f(inp["da_lambda_q2"])[0],
                           f(inp["da_lambda_k2"])[0], f(inp["da_subln_g"])[0], f(inp["wa_sink"])[0]])[None, :]
    sh = dict(
        w_in=f(inp["w_in"])[0], w_mem_kv=f(inp["w_mem_kv"])[0], w_xa_o=f(inp["w_xa_o"])[0], w_da_o=f(inp["w_da_o"])[0],
        w_wa_o=f(inp["w_wa_o"])[0], w_out=f(inp["w_out"])[0], w_router=f(inp["w_router"])[0],
        w_exp_gate=f(inp["w_exp_gate"])[0], w_exp_up=f(inp["w_exp_up"])[0], w_exp_down=f(inp["w_exp_down"])[0],
        gcols=gcols, vecs=np.ascontiguousarray(vecs), gffn=f(inp["ffn_norm_g"]), gfin=f(inp["final_norm_g"])[None, :],
    )
    sh.update(_consts())
    return sh


def kernel(**inp):
    ncores = 8
    x = np.asarray(inp["x"], dtype=np.float32)
    mem = np.asarray(inp["mem"], dtype=np.float32)
    nseq = x.shape[0] // ncores
    if nseq not in _NC_CACHE:
        _NC_CACHE[nseq] = build_nc(nseq)
    nc = _NC_CACHE[nseq]
    sh = _prep_shared(inp)
    in_maps = []
    for c in range(ncores):
        m = dict(sh)
        m["x"] = np.ascontiguousarray(x[c * nseq:(c + 1) * nseq])
        m["mem"] = np.ascontiguousarray(mem[c * nseq:(c + 1) * nseq])
        in_maps.append(m)
    res = run_bass_kernel_spmd(nc, in_maps, core_ids=list(range(ncores)))
    return np.concatenate([r["out"] for r in res.results], axis=0)
```

```python
import numpy as np
import concourse.bass as bass
import concourse.mybir as mybir
from concourse.bass_utils import run_bass_kernel_spmd

F32 = mybir.dt.float32
BF16 = mybir.dt.bfloat16
ALU = mybir.AluOpType
AF = mybir.ActivationFunctionType
AX = mybir.AxisListType

D = 1024
S = 2048
NT = 16
MEM = 256
NE = 16
CAP = 256
DFF = 2048
EPS = 1e-6
C_DAQ, C_DAK, C_DAV, C_WAQ, C_WAK, C_WAV, C_XAQ, C_GATE = 0, 1024, 2048, 3072, 4096, 4352, 4608, 5632
IN_W = 8704
AOFF = 1920
AW = 3968
LAM_INIT = 0.8 - 0.6 * float(np.exp(-0.3 * 0))
ARENA_F32 = 53200


class Buf:
    __slots__ = ("w", "r")

    def __init__(self):
        self.w = None
        self.r = {}


class Builder:
    def __init__(self, nc, sems):
        self.nc = nc
        self.E = {"pe": nc.tensor, "act": nc.scalar, "dve": nc.vector, "pool": nc.gpsimd, "sp": nc.sync}
        self.free_sems = list(sems)
        self.sem = {}
        self.cnt = {}
        self.seen = {e: {} for e in self.E}
        for e in self.E:
            self._newsem(e)

    def _newsem(self, name):
        self.sem[name] = self.free_sems.pop()
        self.cnt[name] = 0

    def _waits(self, eng, reads, writes):
        need = {}
        for b in reads:
            if b.w is not None:
                k, v = b.w
                if need.get(k, 0) < v:
                    need[k] = v
        for b in writes:
            if b.w is not None:
                k, v = b.w
                if need.get(k, 0) < v:
                    need[k] = v
            for k, v in b.r.items():
                if need.get(k, 0) < v:
                    need[k] = v
        seen = self.seen[eng]
        for k, v in need.items():
            if eng == "pe" and k == "pe":
                continue
            if seen.get(k, 0) < v:
                self.E[eng].wait_ge(self.sem[k], v)
                seen[k] = v

    def _mark(self, clock, reads, writes):
        k, v = clock
        for b in reads:
            if b.r.get(k, 0) < v:
                b.r[k] = v
        for b in writes:
            b.w = clock
            b.r = {}

    def op(self, eng, fn, reads=(), writes=(), inc=True):
        self._waits(eng, reads, writes)
        ins = fn()
        if inc:
            self.cnt[eng] += 1
            ins.then_inc(self.sem[eng], 1)
            clock = (eng, self.cnt[eng])
        else:
            clock = (eng, self.cnt[eng] + 1)
        self._mark(clock, reads, writes)
        return ins

    def dma(self, q, out, in_, reads=(), writes=(), sem=None):
        if sem not in self.sem:
            self._newsem(sem)
        self._waits(q, reads, writes)
        ins = self.E[q].dma_start(out=out, in_=in_)
        self.cnt[sem] += 16
        ins.then_inc(self.sem[sem], 16)
        self._mark((sem, self.cnt[sem]), reads, writes)

    def barrier(self, final=False):
        for e in self.E:
            if e == "pool" and not final:
                continue
            seen = self.seen[e]
            for k, v in self.cnt.items():
                if k == e and e == "pe":
                    continue
                if v > 0 and seen.get(k, 0) < v:
                    self.E[e].wait_ge(self.sem[k], v)
                    seen[k] = v

    def mm(self, out, lhsT, rhs, start, stop, reads, writes, inc=None):
        if inc is None:
            inc = stop
        return self.op("pe", lambda: self.nc.tensor.matmul(out, lhsT, rhs, start=start, stop=stop), reads, writes, inc)

    def tr(self, out, in_, ident, reads, writes, inc=True):
        return self.op("pe", lambda: self.nc.tensor.transpose(out, in_, ident), reads, writes, inc)

    def act(self, out, in_, func, reads, writes, **kw):
        return self.op("act", lambda: self.nc.scalar.activation(out=out, in_=in_, func=func, **kw), reads, writes)

    def ts(self, eng, out, in0, s1, s2, op0, op1, reads, writes):
        e = self.E[eng]
        if op1 is None:
            return self.op(eng, lambda: e.tensor_scalar(out, in0, s1, None, op0), reads, writes)
        return self.op(eng, lambda: e.tensor_scalar(out, in0, s1, s2, op0, op1), reads, writes)

    def stt(self, out, in0, scalar, in1, op0, op1, reads, writes, eng="dve"):
        e = self.E[eng]
        return self.op(eng, lambda: e.scalar_tensor_tensor(out, in0, scalar, in1, op0, op1), reads, writes)

    def tt(self, eng, out, in0, in1, op, reads, writes):
        e = self.E[eng]
        return self.op(eng, lambda: e.tensor_tensor(out, in0, in1, op), reads, writes)

    def cp(self, eng, out, in_, reads, writes):
        if eng == "act":
            return self.op("act", lambda: self.nc.scalar.copy(out, in_), reads, writes)
        e = self.E[eng]
        return self.op(eng, lambda: e.tensor_copy(out, in_), reads, writes)


class _Cut(Exception):
    pass


def build_nc(nseq, stop_phase=99, cut=None):
    nc = bass.Bass("TRN2", target_bir_lowering=False)
    dt = lambda name, shape, kind="ExternalInput", d=F32: nc.dram_tensor(name, list(shape), d, kind=kind).ap()
    x = dt("x", [nseq, S, D])
    mem = dt("mem", [nseq, MEM, D])
    w_in = dt("w_in", [D, IN_W])
    w_mem_kv = dt("w_mem_kv", [D, 2 * D])
    w_bo = [dt("w_xa_o", [D, D]), dt("w_da_o", [D, D]), dt("w_wa_o", [D, D])]
    w_out = dt("w_out", [D, D])
    w_router = dt("w_router", [D, NE])
    w_eg = dt("w_exp_gate", [NE, D, DFF])
    w_eu = dt("w_exp_up", [NE, D, DFF])
    w_ed = dt("w_exp_down", [NE, DFF, D])
    gcols_d = dt("gcols", [128, 24])
    vecs_d = dt("vecs", [1, 64 * 4 + 128 + 16])
    gffn_d = dt("gffn", [1, D])
    gfin_d = dt("gfin", [1, D])
    cA_d = dt("cA", [128, AW])
    cI_d = dt("cI", [128, 128])
    cIota_d = dt("cIota", [128, 256])
    cMneg_d = dt("cMneg", [128, 384])
    cCidx_d = dt("cCidx", [128, 2])
    cOneh_d = dt("cOneh", [16, 2048])
    out = dt("out", [nseq, S, D], kind="ExternalOutput")

    from contextlib import ExitStack
    with ExitStack() as es:
        arena = es.enter_context(nc.sbuf_tensor("arena", [128, ARENA_F32], F32))
        psum = es.enter_context(nc.psum_tensor("psum", [128, 4096], F32))
        sems = [es.enter_context(nc.semaphore("s%d" % i)) for i in range(60)]
        B = Builder(nc, sems)

        def reg(off, shape, dtype=F32):
            n = int(np.prod(shape))
            assert off % 4 == 0
            if dtype == F32:
                assert off // 4 + n <= ARENA_F32, (off, shape)
                ap = arena[:, off // 4: off // 4 + n]
            else:
                assert n % 2 == 0 and off // 4 + n // 2 <= ARENA_F32, (off, shape)
                ap = arena[:, off // 4: off // 4 + n // 2].bitcast(BF16)
            if len(shape) == 2:
                return ap.rearrange("p (a b) -> p a b", a=shape[0])
            if len(shape) == 3:
                return ap.rearrange("p (a b c) -> p a b c", a=shape[0], b=shape[1])
            return ap

        def pbank(b, dtype=F32):
            ap = psum[:, b * 512:(b + 1) * 512]
            return ap if dtype == F32 else ap.bitcast(BF16)

        KB = 1024
        cA = reg(0, [AW])
        o = AW * 4
        identF = reg(o, [128]); o += 512
        identB = reg(o, [128], BF16); o += 256
        iota256 = reg(o, [256]); o += 1024
        mneg = reg(o, [3, 128]); o += 1536
        gsub = reg(o, [128]); o += 512
        lamv = reg(o, [4, 64]); o += 1024
        gcols = reg(o, [24]); o += 96
        cidx = reg(o, [2]); o += 8
        esink = reg(o, [16]); o += 64
        lamc = reg(o, [8]); o += 32
        sinkv = reg(o, [16]); o += 64
        wr32 = reg(o, [8, 16]); o += 512
        epsc = reg(o, [4]); o += 16
        wrhi = reg(o, [8, 16], BF16); o += 256
        wrlo = reg(o, [8, 16], BF16); o += 256
        assert o <= 22 * KB, o
        WSLOT = [reg(22 * KB + i * 8 * KB, [8, 512], BF16) for i in range(4)]
        wbuf = [Buf() for _ in range(4)]
        wctr = [0]
        R_M = 54 * KB
        R_H = 118 * KB
        R_O = 150 * KB
        R_T = 182 * KB

        cst = Buf()
        ld = lambda o_, i_: B.dma("sp", o_, i_, (), (cst,), "c")
        ld(cA, cA_d); ld(identF, cI_d); ld(iota256, cIota_d)
        ld(mneg.rearrange("p a b -> p (a b)"), cMneg_d); ld(cidx, cCidx_d); ld(gcols, gcols_d)
        ld(lamv.rearrange("p a b -> p (a b)"), vecs_d[:, 0:256].partition_broadcast(128))
        ld(gsub, vecs_d[:, 256:384].partition_broadcast(128))
        ld(sinkv, vecs_d[:, 384:400].partition_broadcast(128))
        ld(wr32, w_router.rearrange("(c p) n -> p c n", p=128))
        B.cp("dve", identB, identF, (cst,), (cst,))
        B.op("dve", lambda: nc.vector.memset(epsc[:, 0:1], EPS * D), (), (cst,))
        B.op("dve", lambda: nc.vector.memset(epsc[:, 1:2], EPS * 128), (), (cst,))
        B.op("dve", lambda: nc.vector.memset(epsc[:, 2:3], EPS), (), (cst,))
        junk64 = reg(R_T, [64])
        for i in range(2):
            B.tt("dve", junk64, lamv[:, 2 * i, :], lamv[:, 2 * i + 1, :], ALU.mult, (cst,), (cst,))
            B.op("dve", lambda i=i: nc.vector.reduce_sum(lamc[:, i:i + 1], junk64, axis=AX.X), (cst,), (cst,))
        B.act(lamc[:, 0:2], lamc[:, 0:2], AF.Exp, (cst,), (cst,))
        B.tt("dve", lamc[:, 2:3], lamc[:, 0:1], lamc[:, 1:2], ALU.subtract, (cst,), (cst,))
        B.ts("dve", lamc[:, 3:4], lamc[:, 2:3], LAM_INIT, -1.0, ALU.add, ALU.mult, (cst,), (cst,))
        B.act(esink, sinkv, AF.Exp, (cst,), (cst,))
        B.ts("dve", gsub, gsub, 1.0 - LAM_INIT, None, ALU.mult, None, (cst,), (cst,))
        B.cp("dve", wrhi, wr32, (cst,), (cst,))
        B.tt("dve", wr32, wr32, wrhi, ALU.subtract, (cst,), (cst,))
        B.cp("dve", wrlo, wr32, (cst,), (cst,))
        B.barrier()

        def wload(pieces):
            i = wctr[0] % 4
            wctr[0] += 1
            for (src, coff, nchunk) in pieces:
                ncols = src.shape[-1]
                if nchunk == 8:
                    dst = WSLOT[i][:, :, coff:coff + ncols]
                else:
                    dst = WSLOT[i].rearrange("p a b -> p (a b)").rearrange("p (a b) -> p a b", a=16)[:, :, coff:coff + ncols]
                B.dma("pool", dst, src.rearrange("(c p) n -> p c n", p=128), (), (wbuf[i],), "w%d" % i)
            return WSLOT[i], wbuf[i]

        def rms_rstd(ssq, rs, n, reads, writes):
            k = 0 if n == D else 1
            B.act(rs, ssq, AF.Sqrt, tuple(reads) + (cst,), writes, bias=epsc[:, k:k + 1], scale=1.0)
            B.op("dve", lambda: nc.vector.reciprocal(rs, rs), writes, writes)

        def norm_transpose(src_dram_rows, ntile, gcol0, dstT, dstbuf, tmp_off, sp_):
            xt = [reg(tmp_off + i * 4096, [1024]) for i in range(2)]
            xn = [reg(tmp_off + 8192 + i * 2048, [1024], BF16) for i in range(2)]
            sq = reg(tmp_off + 12288, [1024], BF16)
            st = reg(tmp_off + 14336, [8])
            bx = [Buf(), Buf()]; bn = [Buf(), Buf()]; bsq = Buf(); bst = [Buf(), Buf()]
            bp = [Buf(), Buf()]
            for t in range(ntile):
                i = t % 2
                B.dma("sp", xt[i], src_dram_rows[t * 128:(t + 1) * 128, :], (), (bx[i],), sp_ + str(i))
                B.act(sq, xt[i], AF.Square, (bx[i],), (bsq, bst[i]), accum_out=st[:, i:i + 1])
                rms_rstd(st[:, i:i + 1], st[:, 2 + i:3 + i], D, (bst[i],), (bst[i],))
                B.ts("dve", xn[i], xt[i], st[:, 2 + i:3 + i], 32.0, ALU.mult, ALU.mult, (bx[i], bst[i]), (bn[i],))
                pt = pbank(i, BF16).rearrange("p (c n) -> p c n", c=8)
                for c in range(8):
                    B.tr(pt[:, c, :], xn[i][:, c * 128:(c + 1) * 128], identB, (bn[i],), (bp[i],), inc=(c == 7))
                for c in range(8):
                    B.act(dstT[:, c, t * 128:(t + 1) * 128], pt[:, c, :], AF.Copy, (bp[i],), (dstbuf,),
                          scale=gcols[:, gcol0 + c:gcol0 + c + 1])

        def proj_fm(W, wb, wcol, ncol_chunks, srcT, srcbuf, ntok, dst_fn, dstbuf, banks=(6, 7), ctr=[0]):
            pb = [Buf(), Buf()]
            for cc in range(ncol_chunks):
                for t0 in range(0, ntok, 512):
                    n = min(512, ntok - t0)
                    j = ctr[0] % 2; ctr[0] += 1
                    ps = pbank(banks[j])[:, 0:n]
                    for kc in range(8):
                        B.mm(ps, W[:, kc, wcol + cc * 128: wcol + (cc + 1) * 128], srcT[:, kc, t0:t0 + n],
                             kc == 0, kc == 7, (wb, srcbuf), (PB[banks[j]],))
                    B.cp("act", dst_fn(cc, t0, n), ps, (PB[banks[j]],), (dstbuf,))

        PB = [Buf() for _ in range(8)]

        cur_seq = [0]

        def maybe_cut(name):
            if cut == name or cut == name + '@' + str(cur_seq[0]):
                B.barrier()
                print('CUT at', name, B.cnt)
                raise _Cut()

        try:
          for seq in range(nseq):
            cur_seq[0] = seq
            hT = reg(R_H, [8, S], BF16); bhT = Buf()
            oT = reg(R_O, [8, S], BF16); boT = Buf()
            mT_ = reg(R_M, [8, S]); bmerged = Buf()

            maybe_cut('c0')
            norm_transpose(x[seq], NT, 0, hT, bhT, R_M, 'x')
            maybe_cut('p1')
            TM = R_M + 16 * KB
            mT = reg(TM, [8, MEM], BF16); bmT = Buf()
            norm_transpose(mem[seq], 2, 8, mT, bmT, R_M + 44 * KB, 'm')
            mkT = reg(TM + 4 * KB, [8, MEM], BF16); bmk = Buf()
            mv = reg(TM + 8 * KB, [2, 4, 258], BF16); bmv = Buf()
            XQT = reg(TM + 13 * KB, [2, S], BF16); bxq = Buf()
            PTx = [reg(TM + 21 * KB + i * 2048, [2, 512], BF16) for i in range(2)]; bptx = [Buf(), Buf()]
            ox4 = [reg(TM + 25 * KB + i * 512, [256], BF16) for i in range(4)]; box4 = [Buf() for _ in range(4)]
            rx = reg(TM + 27 * KB, [4]); brx4 = [Buf() for _ in range(4)]
            B.op("dve", lambda: nc.vector.memset(mv[:, :, :, 256:258], 1.0), (), (bmv,))
            for half in range(2):
                W, wb = wload([(w_mem_kv[:, half * 512:(half + 1) * 512], 0, 8)])
                proj_fm(W, wb, 0, 4, mT, bmT, MEM, lambda cc, t0, n, half=half: mkT[:, half * 4 + cc, t0:t0 + n], bmk)
            for half in range(2):
                W, wb = wload([(w_mem_kv[:, D + half * 512: D + (half + 1) * 512], 0, 8)])
                for mt in range(2):
                    ps = pbank(6 + mt)
                    for kc in range(8):
                        B.mm(ps, mT[:, kc, mt * 128:(mt + 1) * 128], W[:, kc, :], kc == 0, kc == 7, (wb, bmT), (PB[6 + mt],))
                    B.cp("act", mv[:, mt, 2 * half:2 * half + 2, 0:256], ps.rearrange("p (a b) -> p a b", a=2),
                         (PB[6 + mt],), (bmv,))
            pcx = 0
            for hd in range(4):
                W, wb = wload([(w_in[:, C_XAQ + hd * 256: C_XAQ + (hd + 1) * 256], 0, 8)])
                proj_fm(W, wb, 0, 2, hT, bhT, S, lambda cc, t0, n: XQT[:, cc, t0:t0 + n], bxq)
                for tg in range(4):
                    j = tg % 2
                    for mt in range(2):
                        ps = pbank(mt)
                        for dc in range(2):
                            B.mm(ps, mkT[:, hd * 2 + dc, mt * 128:(mt + 1) * 128], XQT[:, dc, tg * 512:(tg + 1) * 512],
                                 dc == 0, dc == 1, (bmk, bxq), (PB[mt],))
                        B.act(PTx[j][:, mt, :], ps, AF.Exp, (PB[mt],), (bptx[j],), scale=1.0 / 16.0)
                    accs = [psum[:, (2 + qs) * 512:(2 + qs) * 512 + 257] for qs in range(4)]
                    for qs in range(4):
                        for mt in range(2):
                            B.mm(accs[qs], PTx[j][:, mt, qs * 128:(qs + 1) * 128], mv[:, mt, hd, 0:257], mt == 0, mt == 1,
                                 (bptx[j], bmv), (PB[2 + qs],))
                    for qs in range(4):
                        B.op("dve", lambda qs=qs: nc.vector.reciprocal(rx[:, qs:qs + 1], accs[qs][:, 256:257]),
                             (PB[2 + qs],), (brx4[qs],))
                    for qs in range(4):
                        B.act(ox4[qs], accs[qs][:, 0:256], AF.Copy, (PB[2 + qs], brx4[qs]), (box4[qs],), scale=rx[:, qs:qs + 1])
                    pt = pbank(6 + tg % 2, BF16)
                    for qs in range(4):
                        for c in range(2):
                            B.tr(pt[:, qs * 256 + c * 128:qs * 256 + (c + 1) * 128], ox4[qs][:, c * 128:(c + 1) * 128], identB,
                                 (box4[qs],), (PB[6 + tg % 2],), inc=(qs == 3 and c == 1))
                    ptv = pt.rearrange("p (q c n) -> p q c n", q=4, c=2)
                    for c in range(2):
                        B.cp("dve", oT[:, hd * 2 + c, tg * 512:(tg + 1) * 512].rearrange("p (q n) -> p q n", q=4),
                             ptv[:, :, c, :], (PB[6 + tg % 2],), (boT,))
            B.barrier()

            def branch_out(b):
                sg = [reg(R_T + i * 2048, [512]) for i in range(2)]; bsg = [Buf(), Buf()]
                tmp = [reg(R_T + 4096 + i * 2048, [512]) for i in range(2)]; btmp = [Buf(), Buf()]
                n = 0
                for cc in range(8):
                    W, wb = wload([(w_bo[b][:, cc * 128:(cc + 1) * 128], 0, 8),
                                   (w_in[:, C_GATE + {0: 2, 1: 0, 2: 1}[b] * D + cc * 128: C_GATE + {0: 2, 1: 0, 2: 1}[b] * D + (cc + 1) * 128], 128, 8)])
                    for tg in range(4):
                        j = n % 2; n += 1
                        psP = pbank(j); psG = pbank(2 + j)
                        for kc in range(8):
                            B.mm(psG, W[:, kc, 128:256], hT[:, kc, tg * 512:(tg + 1) * 512], kc == 0, kc == 7, (wb, bhT), (PB[2 + j],))
                        for kc in range(8):
                            B.mm(psP, W[:, kc, 0:128], oT[:, kc, tg * 512:(tg + 1) * 512], kc == 0, kc == 7, (wb, boT), (PB[j],))
                        B.act(sg[j], psG, AF.Sigmoid, (PB[2 + j],), (bsg[j],))
                        dst = mT_[:, cc, tg * 512:(tg + 1) * 512]
                        if b == 0:
                            B.tt("dve", dst, psP, sg[j], ALU.mult, (PB[j], bsg[j]), (bmerged,))
                        else:
                            B.tt("dve", tmp[j], psP, sg[j], ALU.mult, (PB[j], bsg[j]), (btmp[j],))
                            B.tt("dve", dst, dst, tmp[j], ALU.add, (btmp[j], bmerged), (bmerged,))

            maybe_cut('xa')
            branch_out(0)
            B.barrier()
            maybe_cut('bo0')
            if stop_phase >= 2:
                QT = reg(R_T, [S], BF16); bq = Buf()
                KT = reg(R_T + 4 * KB, [S], BF16); bk = Buf()
                VA = reg(R_T + 8 * KB, [NT, 130], BF16); bv = Buf()
                o_ = R_T + 8 * KB + 4160
                PT = [reg(o_ + i * 1024, [512], BF16) for i in range(4)]; bpt = [Buf() for _ in range(4)]
                o_ += 4096
                o1 = [reg(o_ + i * 512, [128]) for i in range(4)]; bo1 = [Buf() for _ in range(4)]
                o2 = [reg(o_ + 2048 + i * 512, [128]) for i in range(4)]; bo2 = [Buf() for _ in range(4)]
                on = [reg(o_ + 4096 + i * 256, [128], BF16) for i in range(4)]; bon = [Buf() for _ in range(4)]
                sqj = [reg(o_ + 5120 + i * 256, [128], BF16) for i in range(2)]; bsqj = [Buf(), Buf()]
                stv4 = [reg(o_ + 5632 + i * 32, [8]) for i in range(4)]; bstv4 = [Buf() for _ in range(4)]
                assert o_ + 5760 <= ARENA_F32 * 4, o_
                B.op("dve", lambda: nc.vector.memset(VA[:, :, 128:130], 1.0), (), (bv,))
                _ab1 = Buf()
                accb = [_ab1 for _ in range(8)]
                accap = [psum[:, 2048 + (i // 3) * 512 + (i % 3) * 160: 2048 + (i // 3) * 512 + (i % 3) * 160 + 129] for i in range(8)]
                pair_n = [0]
                def da_w(h):
                    return wload([(w_in[:, C_DAQ + h * 128: C_DAQ + (h + 1) * 128], 0, 8),
                                  (w_in[:, C_DAK + h * 128: C_DAK + (h + 1) * 128], 128, 8),
                                  (w_in[:, C_DAV + h * 128: C_DAV + (h + 1) * 128], 256, 8)])
                da_next = da_w(0)
                for h in range(8):
                    W, wb = da_next
                    if h + 1 < 8:
                        da_next = da_w(h + 1)
                    proj_fm(W, wb, 0, 1, hT, bhT, S, lambda cc, t0, n: QT[:, t0:t0 + n], bq, banks=(0, 1))
                    proj_fm(W, wb, 128, 1, hT, bhT, S, lambda cc, t0, n: KT[:, t0:t0 + n], bk, banks=(0, 1))
                    for t4 in range(4):
                        ps = pbank(2 + t4 % 2)
                        for tt_ in range(4):
                            t = t4 * 4 + tt_
                            for kc in range(8):
                                B.mm(ps[:, tt_ * 128:(tt_ + 1) * 128], hT[:, kc, t * 128:(t + 1) * 128], W[:, kc, 256:384],
                                     kc == 0, kc == 7, (wb, bhT), (PB[2 + t4 % 2],), inc=(kc == 7 and tt_ == 3))
                        B.cp("act", VA[:, t4 * 4:(t4 + 1) * 4, 0:128], ps.rearrange("p (a b) -> p a b", a=4),
                             (PB[2 + t4 % 2],), (bv,))
                    slope = 2.0 ** (-(h + 1))
                    def qk_mid(qg, kb):
                        st_ = pair_n[0] % 2
                        s0 = 2 * pair_n[0]
                        pair_n[0] += 1
                        for m in range(2):
                            B.mm(pbank(st_ * 2 + m), KT[m * 64:(m + 1) * 64, kb * 128:(kb + 1) * 128],
                                 QT[m * 64:(m + 1) * 64, qg * 512:(qg + 1) * 512], True, True, (bk, bq), (PB[st_ * 2 + m],))
                        off = qg * 512 - kb * 128 + AOFF
                        for m in range(2):
                            s_ = s0 + m
                            B.stt(pbank(st_ * 2 + m), cA[:, off:off + 512], -slope * 8.0, pbank(st_ * 2 + m), ALU.mult, ALU.add,
                                  (PB[st_ * 2 + m],), (PB[st_ * 2 + m],))
                            B.act(PT[s_ % 4], pbank(st_ * 2 + m), AF.Exp, (PB[st_ * 2 + m],), (bpt[s_ % 4],), scale=0.125)
                        return s0

                    def pv(kb, s0):
                        for m in range(2):
                            s_ = s0 + m
                            for qs in range(4):
                                a = m * 4 + qs
                                B.mm(accap[a], PT[s_ % 4][:, qs * 128:(qs + 1) * 128], VA[:, kb, 0:129], kb == 0 and a in (0, 3, 6),
                                     kb == 15 and a in (2, 5, 7), (bpt[s_ % 4], bv), (accb[a],), inc=(qs == 3))

                    def norm(qg):
                        Q4 = range(4)
                        for qs in Q4:
                            B.op("dve", lambda qs=qs: nc.vector.reciprocal(stv4[qs][:, 0:1], accap[qs][:, 128:129]), (accb[qs],), (bstv4[qs],))
                            B.op("dve", lambda qs=qs: nc.vector.reciprocal(stv4[qs][:, 1:2], accap[4 + qs][:, 128:129]), (accb[4 + qs],), (bstv4[qs],))
                        for qs in Q4:
                            B.tt("dve", stv4[qs][:, 1:2], stv4[qs][:, 1:2], lamc[:, 3:4], ALU.mult, (bstv4[qs],), (bstv4[qs],))
                        for qs in Q4:
                            B.act(o1[qs], accap[qs][:, 0:128], AF.Copy, (accb[qs], bstv4[qs]), (bo1[qs],), scale=stv4[qs][:, 0:1])
                        for qs in Q4:
                            B.stt(o2[qs], accap[4 + qs][:, 0:128], stv4[qs][:, 1:2], o1[qs], ALU.mult, ALU.add,
                                  (accb[4 + qs], bstv4[qs], bo1[qs]), (bo2[qs],))
                        for qs in Q4:
                            B.act(sqj[qs % 2], o2[qs], AF.Square, (bo2[qs],), (bsqj[qs % 2], bstv4[qs]), accum_out=stv4[qs][:, 2:3])
                        for qs in Q4:
                            B.act(stv4[qs][:, 3:4], stv4[qs][:, 2:3], AF.Sqrt, (bstv4[qs], cst), (bstv4[qs],), bias=epsc[:, 2:3], scale=1.0 / 128.0)
                        for qs in Q4:
                            B.op("dve", lambda qs=qs: nc.vector.reciprocal(stv4[qs][:, 4:5], stv4[qs][:, 3:4]), (bstv4[qs],), (bstv4[qs],))
                        for qs in Q4:
                            B.stt(on[qs], o2[qs], stv4[qs][:, 4:5], gsub, ALU.mult, ALU.mult, (bo2[qs], bstv4[qs]), (bon[qs],))
                        pt = pbank(7, BF16)
                        for qs in Q4:
                            B.tr(pt[:, qs * 128:(qs + 1) * 128], on[qs], identB, (bon[qs],), (PB[7],), inc=(qs == 3))
                        B.cp("act", oT[:, h, qg * 512:(qg + 1) * 512], pt[:, 0:512], (PB[7],), (boT,))

                    pend = []
                    for qg in range(4):
                        if qg == 0:
                            pend = [qk_mid(0, 0), qk_mid(0, 1)]
                        for kb in range(16):
                            pv(kb, pend.pop(0))
                            if kb + 2 < 16:
                                pend.append(qk_mid(qg, kb + 2))
                        if qg + 1 < 4:
                            pend = [qk_mid(qg + 1, 0), qk_mid(qg + 1, 1)]
                        norm(qg)
                B.barrier()
                maybe_cut('da')
                branch_out(1)
                B.barrier()
                maybe_cut('bo1')
            if stop_phase >= 3:
                QW = reg(R_T, [2, S], BF16); bqw = Buf()
                KW = reg(R_T + 8 * KB, [S], BF16); bkw = Buf()
                VW = reg(R_T + 12 * KB, [NT, 66], BF16); bvw = Buf()
                o_ = R_T + 12 * KB + 2112
                WB_ = reg(o_, [3, 512]); bwb = Buf(); o_ += 6144
                STw = reg(o_, [512]); bstw = Buf(); o_ += 2048
                PTw = [reg(o_ + i * 1024, [512], BF16) for i in range(2)]; bptw = [Buf(), Buf()]; o_ += 2048
                ow2 = [reg(o_ + i * 512, [256], BF16) for i in range(2)]; bow2 = [Buf(), Buf()]; o_ += 1024
                rw2 = [reg(o_ + i * 32, [8]) for i in range(2)]; brw2 = [Buf(), Buf()]; o_ += 64
                assert o_ <= ARENA_F32 * 4, o_
                B.op("dve", lambda: nc.vector.memset(VW[:, :, 64:66], 1.0), (), (bvw,))
                for kv in range(4):
                    W, wb = wload([(w_in[:, C_WAQ + kv * 256: C_WAQ + (kv + 1) * 256], 0, 8),
                                   (w_in[:, C_WAK + kv * 64: C_WAK + (kv + 1) * 64], 256, 8),
                                   (w_in[:, C_WAK + kv * 64: C_WAK + (kv + 1) * 64], 320, 8),
                                   (w_in[:, C_WAV + kv * 64: C_WAV + (kv + 1) * 64], 384, 8)])
                    proj_fm(W, wb, 0, 2, hT, bhT, S, lambda cc, t0, n: QW[:, cc, t0:t0 + n], bqw)
                    proj_fm(W, wb, 256, 1, hT, bhT, S, lambda cc, t0, n: KW[:, t0:t0 + n], bkw)
                    for t8 in range(2):
                        ps = pbank(6 + t8)
                        for tt_ in range(8):
                            t = t8 * 8 + tt_
                            for kc in range(8):
                                B.mm(ps[:, tt_ * 64:(tt_ + 1) * 64], hT[:, kc, t * 128:(t + 1) * 128], W[:, kc, 384:448],
                                     kc == 0, kc == 7, (wb, bhT), (PB[6 + t8],), inc=(kc == 7 and tt_ == 7))
                        B.cp("act", VW[:, t8 * 8:(t8 + 1) * 8, 0:64], ps.rearrange("p (a b) -> p a b", a=8), (PB[6 + t8],), (bvw,))
                    maybe_cut('wproj')
                    for di, dlt in enumerate((-1, 0, 1)):
                        for g in range(4):
                            slope = 2.0 ** (-8.0 * (kv * 4 + g + 1) / 16.0)
                            B.stt(WB_[:, di, ((g % 2) * 2 + g // 2) * 128:((g % 2) * 2 + g // 2 + 1) * 128], cA[:, AOFF + 128 * dlt: AOFF + 128 * dlt + 128], -slope * 8.0,
                                  mneg[:, di, :], ALU.mult, ALU.add, (cst, bptw[0], bptw[1]), (bwb,))
                    wsteps = []
                    for i in range(16):
                        es_ = [e for e in (-1, 0, 1) if 0 <= i + e < 16]
                        for ei, e in enumerate(es_):
                            wsteps.append((i, ei, e, ei == len(es_) - 1))

                    def w_qk_mid(n):
                        i, ei, e, last = wsteps[n]
                        kb = i + e
                        di = 1 - e
                        j = n % 2
                        pse = pbank(j); pso = pbank(6 + j)
                        for g in range(4):
                            pg = (pse, pso)[g % 2]
                            B.mm(pg[:, (g // 2) * 128:(g // 2 + 1) * 128], KW[(g % 2) * 64:(g % 2) * 64 + 64, kb * 128:(kb + 1) * 128],
                                 QW[(g % 2) * 64:(g % 2) * 64 + 64, g // 2, i * 128:(i + 1) * 128], True, True,
                                 (bkw, bqw), (PB[j], PB[6 + j]), inc=(g == 3))
                        B.tt("dve", STw[:, 0:256], pse[:, 0:256], WB_[:, di, 0:256], ALU.add, (PB[j], bwb), (bstw,))
                        B.tt("dve", STw[:, 256:512], pso[:, 0:256], WB_[:, di, 256:512], ALU.add, (PB[6 + j], bwb), (bstw,))
                        B.act(PTw[j], STw, AF.Exp, (bstw,), (bptw[j],), scale=0.125)

                    def w_pv(n):
                        i, ei, e, last = wsteps[n]
                        kb = i + e
                        j = n % 2
                        acc = psum[:, 1024 + (i % 2) * 512:1024 + (i % 2) * 512 + 320].rearrange("p (g d) -> p g d", g=4)[:, :, 0:65]
                        ab = PB[2 + i % 2]
                        for g in range(4):
                            p_ = (g % 2) * 2 + g // 2
                            B.mm(acc[:, g, :], PTw[j][:, p_ * 128:(p_ + 1) * 128], VW[:, kb, 0:65], ei == 0 and g == 0, last and g == 3,
                                 (bptw[j], bvw), (ab,), inc=(g == 3))

                    def w_norm(i):
                        acc = psum[:, 1024 + (i % 2) * 512:1024 + (i % 2) * 512 + 320].rearrange("p (g d) -> p g d", g=4)[:, :, 0:65]
                        ab = PB[2 + i % 2]
                        r_ = rw2[i % 2]; br = brw2[i % 2]
                        o_w = ow2[i % 2]; bo_ = bow2[i % 2]
                        B.tt("dve", r_[:, 0:4], acc[:, :, 64], esink[:, kv * 4:kv * 4 + 4], ALU.add, (ab, cst), (br,))
                        B.op("dve", lambda: nc.vector.reciprocal(r_[:, 4:8], r_[:, 0:4]), (br,), (br,))
                        for g in range(4):
                            B.act(o_w[:, g * 64:(g + 1) * 64], acc[:, g, 0:64], AF.Copy, (ab, br), (bo_,), scale=r_[:, 4 + g:5 + g])
                        pt = pbank(4 + i % 2, BF16)
                        for c in range(2):
                            B.tr(pt[:, c * 128:(c + 1) * 128], o_w[:, c * 128:(c + 1) * 128], identB, (bo_,), (PB[4 + i % 2],), inc=(c == 1))
                        B.cp("act", oT[:, kv * 2:kv * 2 + 2, i * 128:(i + 1) * 128],
                             pt[:, 0:256].rearrange("p (c n) -> p c n", c=2), (PB[4 + i % 2],), (boT,))

                    w_qk_mid(0)
                    for n in range(len(wsteps)):
                        if n + 1 < len(wsteps):
                            w_qk_mid(n + 1)
                        w_pv(n)
                        if wsteps[n][3]:
                            w_norm(wsteps[n][0])
                B.barrier()
                maybe_cut('wa')
                branch_out(2)
                B.barrier()

            x1 = reg(R_H, [NT, D]); bx1 = [Buf() for _ in range(NT)]
            xt = [reg(R_T + i * 4096, [1024]) for i in range(2)]; bxt = [Buf(), Buf()]
            mb = [reg(R_T + 8192 + i * 2048, [8, 128], BF16) for i in range(2)]; bmb = [Buf(), Buf()]
            Wo = []
            for half in range(2):
                Wo.append(wload([(w_out[:, half * 512:(half + 1) * 512], 0, 8)]))
            for t in range(NT):
                i = t % 2
                B.dma("sp", xt[i], x[seq, t * 128:(t + 1) * 128, :], (), (bxt[i],), "x%d" % i)
                B.cp("act", mb[i], mT_[:, :, t * 128:(t + 1) * 128], (bmerged,), (bmb[i],))
                for half in range(2):
                    ps = pbank(2 * i + half)
                    for kc in range(8):
                        B.mm(ps, mb[i][:, kc, :], Wo[half][0][:, kc, :], kc == 0, kc == 7, (bmb[i], Wo[half][1]), (PB[2 * i + half],))
                    B.tt("dve", x1[:, t, half * 512:(half + 1) * 512], ps, xt[i][:, half * 512:(half + 1) * 512], ALU.add,
                         (PB[2 * i + half], bxt[i]), (bx1[t],))
            B.barrier()

            maybe_cut('x1')
            h2 = reg(R_M, [NT, D], BF16); bh2 = Buf()
            posm = reg(R_M + 32 * KB, [S]); bposm = Buf()
            SM = R_M + 40 * KB
            afft = reg(SM, [NT, NE]); baff = Buf()
            aff3 = reg(SM + 1024, [NT * NE, 3], BF16); baff3 = Buf()
            posT = reg(SM + 2560, [NT, NE]); bposT = Buf()
            gsel = reg(SM + 3584, [2]); bg = Buf()
            sst = reg(SM + 3600, [16]); bsst = Buf()
            mx8 = reg(SM + 3664, [8]); bmx = Buf()
            affr = reg(SM + 3712, [NT * NE]); baffr = Buf()
            EA = R_M + 48 * KB
            affE = reg(EA, [S]); baffE = Buf()
            wrk = reg(EA + 8 * KB, [S]); bwrk = Buf()
            cs = [reg(R_T + i * 8 * KB, [S]) for i in range(2)]; bcs = [Buf(), Buf()]
            gbc = reg(R_T, [D]); bgbc = Buf()
            h2f = reg(R_T + 4 * KB, [D]); bh2f = Buf()
            TT = R_T + 8 * KB
            lo = reg(TT, [D], BF16); blo = Buf()
            h2T = [reg(TT + 2048 + i * 2048, [8, 128], BF16) for i in range(2)]; bh2T = [Buf(), Buf()]
            sqx = reg(TT + 6144, [D], BF16); bsqx = Buf()
            lg = reg(TT + 8192, [NE]); blg = Buf()
            B.dma("sp", gbc, gffn_d.partition_broadcast(128), (), (bgbc,), "c")
            for t in range(NT):
                B.act(sqx, x1[:, t, :], AF.Square, (bx1[t],), (bsqx, bsst), accum_out=sst[:, 0:1])
                rms_rstd(sst[:, 0:1], sst[:, 1:2], D, (bsst,), (bsst,))
                B.ts("dve", sst[:, 1:2], sst[:, 1:2], 32.0, None, ALU.mult, None, (bsst,), (bsst,))
                B.stt(h2f, x1[:, t, :], sst[:, 1:2], gbc, ALU.mult, ALU.mult, (bx1[t], bsst, bgbc), (bh2f,))
                B.cp("act", h2[:, t, :], h2f, (bh2f,), (bh2,))
                B.tt("dve", lo, h2f, h2[:, t, :], ALU.subtract, (bh2f, bh2), (blo,))
                for k2, src in enumerate((h2[:, t, :], lo)):
                    pt = pbank(k2, BF16).rearrange("p (c n) -> p c n", c=8)
                    for c in range(8):
                        B.tr(pt[:, c, :], src[:, c * 128:(c + 1) * 128], identB, (bh2, blo), (PB[k2],), inc=(c == 7))
                    B.cp("act" if k2 == 0 else "dve", h2T[k2], pt, (PB[k2],), (bh2T[k2],))
                ps = psum[:, 1024:1024 + NE]
                combos = [(0, wrhi), (0, wrlo), (1, wrhi)]
                for ci, (k2, wr) in enumerate(combos):
                    for kc in range(8):
                        B.mm(ps, h2T[k2][:, kc, :], wr[:, kc, :], ci == 0 and kc == 0, ci == 2 and kc == 7,
                             (bh2T[k2], cst), (PB[2],))
                B.op("dve", lambda: nc.vector.reduce_max(sst[:, 2:3], ps, axis=AX.X), (PB[2],), (bsst,))
                B.ts("dve", sst[:, 2:3], sst[:, 2:3], -1.0, None, ALU.mult, None, (bsst,), (bsst,))
                B.act(lg, ps, AF.Exp, (PB[2], bsst), (blg, bsst), bias=sst[:, 2:3], accum_out=sst[:, 3:4])
                B.op("dve", lambda: nc.vector.reciprocal(sst[:, 4:5], sst[:, 3:4]), (bsst,), (bsst,))
                B.ts("dve", afft[:, t, :], lg, sst[:, 4:5], None, ALU.mult, None, (blg, bsst), (baff,))
                pt = psum[0:16, 1536:1536 + 128]
                B.tr(pt, afft[:, t, :], identF, (baff,), (PB[3],))
                B.cp("act", affE[0:16, t * 128:(t + 1) * 128], pt, (PB[3],), (baffE,))
            affv = afft.rearrange("p a b -> p (a b)")
            B.cp("dve", aff3[:, :, 0], affv, (baff,), (baff3,))
            B.tt("dve", affr, affv, aff3[:, :, 0], ALU.subtract, (baff, baff3), (baffr,))
            B.cp("dve", aff3[:, :, 1], affr, (baffr,), (baff3,))
            B.tt("dve", affr, affr, aff3[:, :, 1], ALU.subtract, (baffr, baff3), (baffr,))
            B.cp("dve", aff3[:, :, 2], affr, (baffr,), (baff3,))
            B.barrier()
            E16 = slice(0, 16)
            B.cp("dve", wrk[E16], affE[E16], (baffE,), (bwrk,))
            for it in range(CAP // 8):
                B.op("dve", lambda: nc.vector.max(out=mx8[E16], in_=wrk[E16]), (bwrk,), (bmx,))
                if it < CAP // 8 - 1:
                    B.op("dve", lambda: nc.vector.match_replace(out=wrk[E16], in_to_replace=mx8[E16], in_values=wrk[E16],
                                                                imm_value=-1.0), (bmx, bwrk), (bwrk,))
            B.op("dve", lambda: nc.vector.tensor_reduce(out=sst[E16, 5:6], in_=mx8[E16], axis=AX.X, op=ALU.min), (bmx,), (bsst,))
            B.ts("dve", wrk[E16], affE[E16], sst[E16, 5:6], None, ALU.is_ge, None, (baffE, bsst), (bwrk,))
            B.cp("dve", cs[0][E16], wrk[E16], (bwrk,), (bcs[0],))
            a = 0
            sh = 1
            while sh < S:
                b_ = 1 - a
                B.cp("act", cs[b_][E16, 0:sh], cs[a][E16, 0:sh], (bcs[a],), (bcs[b_],))
                B.tt("dve", cs[b_][E16, sh:S], cs[a][E16, sh:S], cs[a][E16, 0:S - sh], ALU.add, (bcs[a],), (bcs[b_],))
                a = b_
                sh *= 2
            B.tt("dve", posm[E16], cs[a][E16], wrk[E16], ALU.mult, (bcs[a], bwrk), (bposm,))
            B.ts("dve", posm[E16], posm[E16], -1.0, None, ALU.add, None, (bposm,), (bposm,))
            for t in range(NT):
                pt = psum[:, 1536 + (t % 2) * 512: 1536 + (t % 2) * 512 + 16]
                B.tr(pt, posm[E16, t * 128:(t + 1) * 128], identF[0:16, 0:16], (bposm,), (PB[3 + t % 2],))
                B.cp("act", posT[:, t, :], pt, (PB[3 + t % 2],), (bposT,))
            B.barrier()

            maybe_cut('route')
            Sel = reg(EA, [NT, CAP], BF16); bsel = Buf()
            SelT = reg(EA + 8 * KB, [2, S], BF16); bselT = Buf()
            xeT = reg(R_T, [8, CAP], BF16); bxe = Buf()
            actT = reg(R_T + 4 * KB, [16, CAP], BF16); bact = Buf()
            ye = reg(R_T + 12 * KB, [2, D], BF16); bye = Buf()
            oneh = reg(R_T + 16 * KB, [S]); boneh = Buf()
            sa = [reg(SM + 5120 + i * 1024, [CAP]) for i in range(2)]; bsa = [Buf(), Buf()]
            B.dma("sp", oneh[0:16], cOneh_d, (), (boneh,), "c")
            pn = 0
            def build_sel(e):
                for t in range(NT):
                    B.ts("dve", Sel[:, t, :], iota256, posT[:, t, e:e + 1], None, ALU.is_equal, None, (cst, bposT), (bsel,))

            def build_selT(e):
                for tg in range(4):
                    j = tg % 2
                    ps = pbank(j)
                    B.mm(ps, oneh[0:16, e * 128:(e + 1) * 128], posm[0:16, tg * 512:(tg + 1) * 512], True, True, (boneh, bposm), (PB[j],))
                    for cch in range(2):
                        B.ts("dve", SelT[:, cch, tg * 512:(tg + 1) * 512], ps, cidx[:, cch:cch + 1], None, ALU.is_equal, None,
                             (PB[j], cst), (bselT,))

            def gather_group(dc):
                j = dc % 2
                ps = pbank(j)[:, 0:CAP]
                for t in range(NT):
                    B.mm(ps, h2[:, t, dc * 128:(dc + 1) * 128], Sel[:, t, :], t == 0, t == NT - 1, (bh2, bsel), (PB[j],))
                B.cp("act", xeT[:, dc, :], ps, (PB[j],), (bxe,))

            def g_gather(e):
                for cch in range(2):
                    ps = psum[:, 3584 - cch * 512: 3584 - cch * 512 + 3]
                    for t in range(NT):
                        B.mm(ps, Sel[:, t, cch * 128:(cch + 1) * 128], aff3[:, t * NE + e, :], t == 0, t == NT - 1,
                             (bsel, baff3), (PB[7 - cch],), inc=(t == NT - 1))
                    B.op("dve", lambda ps=ps, cch=cch: nc.vector.reduce_sum(gsel[:, cch:cch + 1], ps, axis=AX.X), (PB[7 - cch],), (bg,))

            gu_state = {}

            def gate_up_group(e, fc, fs):
                if fs == 0:
                    gu_state["g"] = wload([(w_eg[e][:, fc * 512:(fc + 1) * 512], 0, 8)])
                    gu_state["u"] = wload([(w_eu[e][:, fc * 512:(fc + 1) * 512], 0, 8)])
                Wg, wbg = gu_state["g"]; Wu, wbu = gu_state["u"]
                j = (fc * 4 + fs) % 2
                ps = pbank(2 + j)
                for kc in range(8):
                    B.mm(ps[:, 0:CAP], Wg[:, kc, fs * 128:(fs + 1) * 128], xeT[:, kc, :], kc == 0, kc == 7, (wbg, bxe), (PB[2 + j],), inc=False)
                for kc in range(8):
                    B.mm(ps[:, CAP:2 * CAP], Wu[:, kc, fs * 128:(fs + 1) * 128], xeT[:, kc, :], kc == 0, kc == 7, (wbu, bxe), (PB[2 + j],))
                B.act(sa[j], ps[:, 0:CAP], AF.Silu, (PB[2 + j],), (bsa[j],))
                B.tt("dve", actT[:, fc * 4 + fs, :], sa[j], ps[:, CAP:2 * CAP], ALU.mult, (bsa[j], PB[2 + j]), (bact,))

            def down(e):
                for dq in range(4):
                    Wd, wbd = wload([(w_ed[e][:, dq * 256:(dq + 1) * 256], 0, 16)])
                    Wd16 = Wd.rearrange("p a b -> p (a b)").rearrange("p (a b) -> p a b", a=16)
                    for cch in range(2):
                        j = (dq * 2 + cch) % 2
                        ps = pbank(4 + j)[:, 0:256]
                        for fch in range(16):
                            B.mm(ps, actT[:, fch, cch * 128:(cch + 1) * 128], Wd16[:, fch, :], fch == 0, fch == 15, (bact, wbd), (PB[4 + j],))
                        B.act(ye[:, cch, dq * 256:(dq + 1) * 256], ps, AF.Copy, (PB[4 + j], bg), (bye,), scale=gsel[:, cch:cch + 1])

            def scatter_unit(ui):
                t, half = ui // 2, ui % 2
                j = ui % 2
                ps = pbank(6 + j)
                for cch in range(2):
                    B.mm(ps, SelT[:, cch, t * 128:(t + 1) * 128], ye[:, cch, half * 512:(half + 1) * 512], cch == 0, cch == 1,
                         (bselT, bye), (PB[6 + j],))
                B.tt("dve", x1[:, t, half * 512:(half + 1) * 512], x1[:, t, half * 512:(half + 1) * 512], ps, ALU.add,
                     (PB[6 + j], bx1[t]), (bx1[t],))

            build_sel(0)
            for dc in range(8):
                gather_group(dc)
            g_gather(0)
            build_selT(0)
            for fc in range(4):
                for fs in range(4):
                    gate_up_group(0, fc, fs)
            if NE > 1:
                build_sel(1)
            for e in range(NE):
                down(e)
                if e + 1 < NE:
                    for dc in range(8):
                        gather_group(dc)
                    g_gather(e + 1)
                    for k in range(16):
                        scatter_unit(2 * k)
                        gate_up_group(e + 1, k // 4, k % 4)
                        scatter_unit(2 * k + 1)
                    build_selT(e + 1)
                    if e + 2 < NE:
                        build_sel(e + 2)
                else:
                    for ui in range(32):
                        scatter_unit(ui)
            B.barrier()
            maybe_cut('moe')
            gfb = reg(R_T, [D]); bgf = Buf()
            ob = [reg(R_T + 4 * KB + i * 4 * KB, [D]) for i in range(2)]; bob = [Buf(), Buf()]
            sq2 = reg(R_T + 12 * KB, [D], BF16); bsq2 = Buf()
            B.dma("sp", gfb, gfin_d.partition_broadcast(128), (), (bgf,), "c")
            for t in range(NT):
                i = t % 2
                B.act(sq2, x1[:, t, :], AF.Square, (bx1[t],), (bsq2, bsst), accum_out=sst[:, 8:9])
                rms_rstd(sst[:, 8:9], sst[:, 9:10], D, (bsst,), (bsst,))
                B.ts("dve", sst[:, 9:10], sst[:, 9:10], 32.0, None, ALU.mult, None, (bsst,), (bsst,))
                B.stt(ob[i], x1[:, t, :], sst[:, 9:10], gfb, ALU.mult, ALU.mult, (bx1[t], bsst, bgf), (bob[i],))
                B.dma("sp", out[seq, t * 128:(t + 1) * 128, :], ob[i], (bob[i],), (), "o%d" % i)
            B.barrier(final=True)
        except _Cut:
            pass
        build_nc.last_counts = dict(B.cnt)
    return nc


def _consts():
    p = np.arange(128, dtype=np.float32)[:, None]
    m = np.arange(AW, dtype=np.float32)[None, :]
    cA = np.abs(m - p - AOFF).astype(np.float32)
    cI = np.eye(128, dtype=np.float32)
    cIota = np.broadcast_to(np.arange(256, dtype=np.float32)[None, :], (128, 256)).copy()
    j = np.arange(128, dtype=np.float32)[None, :]
    mneg = np.zeros((128, 3, 128), np.float32)
    for di, dlt in enumerate((-1, 0, 1)):
        dist = np.abs(j + 128 * dlt - p)
        mneg[:, di, :] = np.where(dist <= 128, 0.0, -1.0e5)
    cCidx = np.stack([np.arange(128, dtype=np.float32), np.arange(128, dtype=np.float32) + 128], axis=1)
    oneh = np.zeros((16, 2048), np.float32)
    for e in range(16):
        oneh[e, e * 128:(e + 1) * 128] = 1.0
    return dict(cA=cA, cI=cI, cIota=cIota, cMneg=mneg.reshape(128, 384), cCidx=np.ascontiguousarray(cCidx), cOneh=oneh)


_NC_CACHE = {}


def _prep_shared(inp):
    f = lambda a: np.ascontiguousarray(np.asarray(a, dtype=np.float32))
    gcols = np.zeros((128, 24), np.float32)
    gcols[:, 0:8] = f(inp["attn_norm_g"])[0].reshape(8, 128).T
    gcols[:, 8:16] = f(inp["mem_norm_g"])[0].reshape(8, 128).T
    vecs = np.concatenate([f(inp["da_lambda_q1"])[0], f(inp["da_lambda_k1"])[0], f(inp["da_lambda_q2"])[0],
                           f(inp["da_lambda_k2"])[0], f(inp["da_subln_g"])[0], f(inp["wa_sink"])[0]])[None, :]
    sh = dict(
        w_in=f(inp["w_in"])[0], w_mem_kv=f(inp["w_mem_kv"])[0], w_xa_o=f(inp["w_xa_o"])[0], w_da_o=f(inp["w_da_o"])[0],
        w_wa_o=f(inp["w_wa_o"])[0], w_out=f(inp["w_out"])[0], w_router=f(inp["w_router"])[0],
        w_exp_gate=f(inp["w_exp_gate"])[0], w_exp_up=f(inp["w_exp_up"])[0], w_exp_down=f(inp["w_exp_down"])[0],
        gcols=gcols, vecs=np.ascontiguousarray(vecs), gffn=f(inp["ffn_norm_g"]), gfin=f(inp["final_norm_g"])[None, :],
    )
    sh.update(_consts())
    return sh


def kernel(**inp):
    ncores = 8
    x = np.asarray(inp["x"], dtype=np.float32)
    mem = np.asarray(inp["mem"], dtype=np.float32)
    nseq = x.shape[0] // ncores
    if nseq not in _NC_CACHE:
        _NC_CACHE[nseq] = build_nc(nseq)
    nc = _NC_CACHE[nseq]
    sh = _prep_shared(inp)
    in_maps = []
    for c in range(ncores):
        m = dict(sh)
        m["x"] = np.ascontiguousarray(x[c * nseq:(c + 1) * nseq])
        m["mem"] = np.ascontiguousarray(mem[c * nseq:(c + 1) * nseq])
        in_maps.append(m)
    res = run_bass_kernel_spmd(nc, in_maps, core_ids=list(range(ncores)))
    return np.concatenate([r["out"] for r in res.results], axis=0)
```
